# Optimizing a Trainium2 kernel written in Bass

```python
import math
import jax, jax.numpy as jnp
from jax import lax
import numpy as np

D_MODEL = 1024
BATCH = 16
SEQ = 2048
DEPTH = 2

CHUNK = 64
M_HEADS = 8
M_DQK = 64
M_DV = 128
M_QK = M_HEADS * M_DQK
M_V = M_HEADS * M_DV
M_CONV = 4
S_HEADDIM = 64
S_INNER = D_MODEL
S_HEADS = S_INNER // S_HEADDIM
S_GROUPS = 4
S_STATE = 128
S_CONV = 4
S_XBC = S_INNER + 2 * S_GROUPS * S_STATE
D_FF = 2752
FFN_CONV = 3
IN_SIZES = (M_QK, M_QK, M_V, M_V, M_HEADS, M_HEADS, S_INNER, S_XBC, S_HEADS, D_MODEL, D_MODEL)
D_IN = sum(IN_SIZES)
ALPHA = (2 * DEPTH) ** 0.25
BETA = (8 * DEPTH) ** -0.25
LN_EPS = 1e-5
RMS_EPS = 1e-6
NEG_BIG = -1e30

kernel_name = "hybrid_mlstm_mamba2_convffn_deepnorm"


def layer_norm(x, g, b):
    xf = x.astype(jnp.float32)
    mu = jnp.mean(xf, -1, keepdims=True)
    var = jnp.mean(jnp.square(xf - mu), -1, keepdims=True)
    return ((xf - mu) * lax.rsqrt(var + LN_EPS) * g.astype(jnp.float32) + b.astype(jnp.float32)).astype(x.dtype)


def group_rms_norm(x, g, n_groups):
    shp = x.shape
    xf = x.astype(jnp.float32).reshape(shp[:-1] + (n_groups, shp[-1] // n_groups))
    xf = xf * lax.rsqrt(jnp.mean(xf * xf, -1, keepdims=True) + RMS_EPS)
    return (xf.reshape(shp) * g.astype(jnp.float32)).astype(x.dtype)


def causal_dwconv(x, w, b):
    K, C = w.shape
    y = lax.conv_general_dilated(x, w[:, None, :], window_strides=(1,), padding=[(K - 1, 0)],
                                 dimension_numbers=('NWC', 'WIO', 'NWC'), feature_group_count=C)
    return y + b


def mlstm_chunkwise(q, k, v, i_pre, f_pre):
    f32 = jnp.float32
    Bsz, T = q.shape[0], q.shape[1]
    nc = T // CHUNK

    def chunks(a):
        a = a.reshape((Bsz, nc, CHUNK) + a.shape[2:])
        return jnp.moveaxis(a, 3, 1)

    qc = chunks(q.astype(f32))
    kc = chunks(k.astype(f32)) * (M_DQK ** -0.5)
    vc = chunks(v.astype(f32))
    log_i = chunks(i_pre.astype(f32))
    log_f = jax.nn.log_sigmoid(chunks(f_pre.astype(f32)))
    b = jnp.cumsum(log_f, -1)
    g = b[..., -1]

    a = g[..., None] - b + log_i
    m_loc = jnp.max(a, -1)
    w_s = jnp.exp(a - m_loc[..., None])
    kw = kc * w_s[..., None]
    c_loc = jnp.einsum('bhcsk,bhcsv->bhckv', kw, vc)
    n_loc = jnp.sum(kw, 3)

    def step(carry, inp):
        C, n, m = carry
        g_c, m_l, C_l, n_l = inp
        m_new = jnp.maximum(g_c + m, m_l)
        s_prev = jnp.exp(g_c + m - m_new)
        s_loc = jnp.exp(m_l - m_new)
        C_new = s_prev[..., None, None] * C + s_loc[..., None, None] * C_l
        n_new = s_prev[..., None] * n + s_loc[..., None] * n_l
        return (C_new, n_new, m_new), (C, n, m)

    init = (jnp.zeros((Bsz, M_HEADS, M_DQK, M_DV), f32),
            jnp.zeros((Bsz, M_HEADS, M_DQK), f32),
            jnp.full((Bsz, M_HEADS), NEG_BIG, f32))
    xs = (jnp.moveaxis(g, 2, 0), jnp.moveaxis(m_loc, 2, 0),
          jnp.moveaxis(c_loc, 2, 0), jnp.moveaxis(n_loc, 2, 0))
    _, (C_prev, n_prev, m_prev) = lax.scan(step, init, xs)
    C_prev = jnp.moveaxis(C_prev, 0, 2)
    n_prev = jnp.moveaxis(n_prev, 0, 2)
    m_prev = jnp.moveaxis(m_prev, 0, 2)

    causal = jnp.tril(jnp.ones((CHUNK, CHUNK), bool))
    log_d = jnp.where(causal, b[..., :, None] - b[..., None, :] + log_i[..., None, :], -jnp.inf)
    m_inter = b + m_prev[..., None]
    m_comb = jnp.maximum(m_inter, jnp.max(log_d, -1))
    s_inter = jnp.exp(m_inter - m_comb)
    scores = jnp.einsum('bhctk,bhcsk->bhcts', qc, kc) * jnp.exp(log_d - m_comb[..., None])
    num = (jnp.einsum('bhcts,bhcsv->bhctv', scores, vc)
           + s_inter[..., None] * jnp.einsum('bhctk,bhckv->bhctv', qc, C_prev))
    den = jnp.sum(scores, -1) + s_inter * jnp.einsum('bhctk,bhck->bhct', qc, n_prev)
    h = num / jnp.maximum(jnp.abs(den), jnp.exp(-m_comb))[..., None]
    h = jnp.moveaxis(h, 1, 3).reshape(Bsz, T, M_HEADS, M_DV)
    return h.astype(q.dtype)


def segsum_exp(a):
    cs = jnp.cumsum(a, -1)
    L = a.shape[-1]
    causal = jnp.tril(jnp.ones((L, L), bool))
    return jnp.exp(jnp.where(causal, cs[..., :, None] - cs[..., None, :], -jnp.inf))


def ssd_chunked(x, dt, A, Bm, Cm):
    f32 = jnp.float32
    Bsz, T = x.shape[0], x.shape[1]
    nc = T // CHUNK
    R = S_HEADS // S_GROUPS
    dt = dt.astype(f32)
    xc = (x.astype(f32) * dt[..., None]).reshape(Bsz, nc, CHUNK, S_GROUPS, R, S_HEADDIM)
    a = (dt * A.astype(f32)).reshape(Bsz, nc, CHUNK, S_GROUPS, R)
    a = jnp.transpose(a, (0, 3, 4, 1, 2))
    Bc = Bm.astype(f32).reshape(Bsz, nc, CHUNK, S_GROUPS, S_STATE)
    Cc = Cm.astype(f32).reshape(Bsz, nc, CHUNK, S_GROUPS, S_STATE)
    a_cs = jnp.cumsum(a, -1)

    cb = jnp.einsum('bclgn,bcsgn->bgcls', Cc, Bc)
    mix = cb[:, :, None] * segsum_exp(a)
    y_diag = jnp.einsum('bgrcls,bcsgrp->bclgrp', mix, xc)

    decay_to_end = jnp.transpose(jnp.exp(a_cs[..., -1:] - a_cs), (0, 3, 4, 1, 2))
    states = jnp.einsum('bcsgn,bcsgrp->bcgrpn', Bc, xc * decay_to_end[..., None])

    chunk_decay = jnp.exp(a_cs[..., -1])

    def step(h, inp):
        dec, st = inp
        return dec[..., None, None] * h + st, h

    h0 = jnp.zeros((Bsz, S_GROUPS, R, S_HEADDIM, S_STATE), f32)
    _, h_prev = lax.scan(step, h0, (jnp.moveaxis(chunk_decay, 3, 0), jnp.moveaxis(states, 1, 0)))
    h_prev = jnp.moveaxis(h_prev, 0, 1)

    decay_in = jnp.transpose(jnp.exp(a_cs), (0, 3, 4, 1, 2))
    y_off = jnp.einsum('bclgn,bcgrpn->bclgrp', Cc, h_prev) * decay_in[..., None]
    return (y_diag + y_off).reshape(Bsz, T, S_HEADS, S_HEADDIM)


def mlstm_branch(q, k, v, o_pre, i_pre, f_pre, conv_w, conv_b, i_bias, f_bias, norm_g):
    Bsz, T = q.shape[0], q.shape[1]
    qk = jax.nn.silu(causal_dwconv(jnp.concatenate([q, k], -1), conv_w, conv_b))
    q, k = jnp.split(qk, 2, -1)
    h = mlstm_chunkwise(q.reshape(Bsz, T, M_HEADS, M_DQK), k.reshape(Bsz, T, M_HEADS, M_DQK),
                        v.reshape(Bsz, T, M_HEADS, M_DV), i_pre + i_bias, f_pre + f_bias)
    h = group_rms_norm(h.reshape(Bsz, T, M_V), norm_g, M_HEADS)
    return jax.nn.sigmoid(o_pre) * h


def mamba2_branch(z, xbc, dt_raw, conv_w, conv_b, dt_bias, a_log, d_skip, norm_g):
    Bsz, T = z.shape[0], z.shape[1]
    xbc = jax.nn.silu(causal_dwconv(xbc, conv_w, conv_b))
    xs, Bm, Cm = jnp.split(xbc, [S_INNER, S_INNER + S_GROUPS * S_STATE], -1)
    xs = xs.reshape(Bsz, T, S_HEADS, S_HEADDIM)
    Bm = Bm.reshape(Bsz, T, S_GROUPS, S_STATE)
    Cm = Cm.reshape(Bsz, T, S_GROUPS, S_STATE)
    dt = jax.nn.softplus(dt_raw.astype(jnp.float32) + dt_bias.astype(jnp.float32))
    A = -jnp.exp(a_log.astype(jnp.float32))
    y = ssd_chunked(xs, dt, A, Bm, Cm) + d_skip.astype(jnp.float32)[:, None] * xs.astype(jnp.float32)
    y = y.reshape(Bsz, T, S_INNER).astype(z.dtype)
    return group_rms_norm(y * jax.nn.silu(z), norm_g, S_GROUPS)


def setup_inputs(seed: int = 0) -> dict:
    key = jax.random.key(seed)
    ks = jax.random.split(key, 32)
    L = DEPTH
    f32 = jnp.float32

    def nrm(k, shape, scale):
        return scale * jax.random.normal(k, shape, f32)

    dt0 = jnp.exp(jax.random.uniform(ks[12], (L, S_HEADS), f32, math.log(1e-3), math.log(1e-1)))
    return {
        "x": nrm(ks[0], (BATCH, SEQ, D_MODEL), 1.0),
        "in_ln_g": 1.0 + nrm(ks[1], (D_MODEL,), 0.02),
        "in_ln_b": nrm(ks[2], (D_MODEL,), 0.02),
        "w_in": nrm(ks[3], (L, D_MODEL, D_IN), D_MODEL ** -0.5),
        "m_conv_w": nrm(ks[4], (L, M_CONV, 2 * M_QK), M_CONV ** -0.5),
        "m_conv_b": nrm(ks[5], (L, 2 * M_QK), 0.01),
        "m_i_bias": nrm(ks[6], (L, M_HEADS), 0.1),
        "m_f_bias": jnp.linspace(3.0, 6.0, M_HEADS, dtype=f32)[None] + nrm(ks[7], (L, M_HEADS), 0.1),
        "m_norm_g": 1.0 + nrm(ks[8], (L, M_V), 0.02),
        "s_conv_w": nrm(ks[9], (L, S_CONV, S_XBC), S_CONV ** -0.5),
        "s_conv_b": nrm(ks[10], (L, S_XBC), 0.01),
        "s_dt_bias": dt0 + jnp.log(-jnp.expm1(-dt0)),
        "s_a_log": jnp.log(jax.random.uniform(ks[13], (L, S_HEADS), f32, 1.0, 16.0)),
        "s_d": 1.0 + nrm(ks[14], (L, S_HEADS), 0.1),
        "s_norm_g": 1.0 + nrm(ks[15], (L, S_INNER), 0.02),
        "p_a": nrm(ks[16], (L, M_V, D_MODEL), M_V ** -0.5),
        "p_b": nrm(ks[17], (L, S_INNER, D_MODEL), S_INNER ** -0.5),
        "w_out": nrm(ks[18], (L, D_MODEL, D_MODEL), BETA * D_MODEL ** -0.5),
        "ln1_g": 1.0 + nrm(ks[19], (L, D_MODEL), 0.02),
        "ln1_b": nrm(ks[20], (L, D_MODEL), 0.02),
        "w_up": nrm(ks[21], (L, D_MODEL, 2 * D_FF), D_MODEL ** -0.5),
        "f_conv_w": nrm(ks[22], (L, FFN_CONV, 2 * D_FF), FFN_CONV ** -0.5),
        "f_conv_b": nrm(ks[23], (L, 2 * D_FF), 0.01),
        "w_down": nrm(ks[24], (L, D_FF, D_MODEL), BETA * D_FF ** -0.5),
        "ln2_g": 1.0 + nrm(ks[25], (L, D_MODEL), 0.02),
        "ln2_b": nrm(ks[26], (L, D_MODEL), 0.02),
    }


def reference(x, in_ln_g, in_ln_b, w_in, m_conv_w, m_conv_b, m_i_bias, m_f_bias, m_norm_g,
              s_conv_w, s_conv_b, s_dt_bias, s_a_log, s_d, s_norm_g, p_a, p_b, w_out,
              ln1_g, ln1_b, w_up, f_conv_w, f_conv_b, w_down, ln2_g, ln2_b):
    splits = np.cumsum(IN_SIZES)[:-1].tolist()
    x = layer_norm(x, in_ln_g, in_ln_b)
    for l in range(DEPTH):
        proj = x @ w_in[l]
        q, k, v, o_pre, i_pre, f_pre, z, xbc, dt_raw, g_a, g_b = jnp.split(proj, splits, -1)
        h_a = mlstm_branch(q, k, v, o_pre, i_pre, f_pre, m_conv_w[l], m_conv_b[l],
                           m_i_bias[l], m_f_bias[l], m_norm_g[l])
        h_b = mamba2_branch(z, xbc, dt_raw, s_conv_w[l], s_conv_b[l], s_dt_bias[l],
                            s_a_log[l], s_d[l], s_norm_g[l])
        merged = jax.nn.sigmoid(g_a) * (h_a @ p_a[l]) + jax.nn.sigmoid(g_b) * (h_b @ p_b[l])
        x = layer_norm(ALPHA * x + merged @ w_out[l], ln1_g[l], ln1_b[l])
        u = causal_dwconv(x @ w_up[l], f_conv_w[l], f_conv_b[l])
        u_gate, u_val = jnp.split(u, 2, -1)
        x = layer_norm(ALPHA * x + (jax.nn.silu(u_gate) * u_val) @ w_down[l], ln2_g[l], ln2_b[l])
    return x
```

```python
import numpy as np
from contextlib import ExitStack
import concourse.bass as bass
import concourse.mybir as mybir
from concourse.bass_utils import run_bass_kernel_spmd
from concourse.alu_op_type import AluOpType as ALU

AF = mybir.ActivationFunctionType
AX = mybir.AxisListType
F32 = mybir.dt.float32
BF16 = mybir.dt.bfloat16

DEPTH = 2
D = 1024
KT = 8
SEQ = 2048
BATCH = 16
TB = 512
NTT = 4
NBLK = SEQ // TB
M_H, DQK, DV = 8, 64, 128
S_H, S_P, S_G, S_N = 16, 64, 4, 128
DFF = 2752
NJ = 22
D_IN = 8224
ALPHA = (2 * DEPTH) ** 0.25
LN_EPS = 1e-5
RMS_EPS = 1e-6
NEG_BIG = -1e30

ENGS = ("pe", "act", "dve", "pool", "sp")
RELAX = ()
SCHEDULE = True


class DG:
    def __init__(self, name):
        self.name = name
        self.sem = None
        self.count = 0


class Buf:
    def __init__(self, h, name, ncell=1):
        self.h = h
        self.name = name
        self.ncell = ncell
        self.lw = [None] * ncell
        self.rd = [[] for _ in range(ncell)]
        self.dg = None

    def __getitem__(self, k):
        return self.h[k]

    def c(self, *idx):
        return (self, list(idx))


def _cells(x):
    if isinstance(x, Buf):
        return [(x, i) for i in range(x.ncell)]
    b, idx = x
    return [(b, i) for i in idx]


class Op:
    __slots__ = ("eng", "fn", "idx", "deps", "sig", "signo", "dma", "dg", "dval", "cost", "alld")


class Prog:
    def __init__(self, nc, es):
        self.nc = nc
        self.es = es
        self.ops = []
        self.dgs = []
        self.nsig = {e: 0 for e in ENGS}

    def sbuf(self, name, shape, dtype, ncell=1):
        h = self.es.enter_context(self.nc.sbuf_tensor("t_" + name, list(shape), dtype))
        return Buf(h, name, ncell)

    def psum(self, name, shape, dtype, ncell=1):
        h = self.es.enter_context(self.nc.psum_tensor("t_" + name, list(shape), dtype))
        return Buf(h, name, ncell)

    def op(self, eng, fn, reads=(), writes=(), dma=None, cost=0.5):
        o = Op()
        o.eng, o.fn, o.idx = eng, fn, len(self.ops)
        o.cost = cost
        o.sig, o.signo, o.dma, o.dg, o.dval = False, None, dma is not None, None, None
        deps = {}
        for r in reads:
            for (b, i) in _cells(r):
                if b.lw[i] is not None:
                    deps[b.lw[i]] = True
        for w in writes:
            for (b, i) in _cells(w):
                if b.lw[i] is not None:
                    deps.setdefault(b.lw[i], False)
                for j in b.rd[i]:
                    deps.setdefault(j, False)
        deps.pop(o.idx, None)
        o.deps = deps
        for r in reads:
            for (b, i) in _cells(r):
                b.rd[i].append(o.idx)
        for w in writes:
            for (b, i) in _cells(w):
                b.lw[i] = o.idx
                b.rd[i] = []
        if o.dma:
            if dma.dg is None:
                dma.dg = DG(dma.name)
                self.dgs.append(dma.dg)
            o.dg = dma.dg
            o.dg.count += 16
            o.dval = o.dg.count
        self.ops.append(o)
        return o

    def dma(self, eng, out_ap, in_ap, reads=(), writes=(), sem_buf=None):
        if sem_buf is None:
            sem_buf = _cells(writes[0])[0][0] if writes else _cells(reads[0])[0][0]
        nbytes = 1
        for d_ in in_ap.shape:
            nbytes *= d_
        nbytes *= 4 if in_ap.dtype == F32 else 2
        return self.op(eng, lambda e: e.dma_start(out=out_ap, in_=in_ap),
                       reads=reads, writes=writes, dma=sem_buf, cost=2.0 + nbytes / 120e3)

    def fence(self, old, new):
        s = set()
        for b in old:
            for i in range(b.ncell):
                if b.lw[i] is not None:
                    s.add(b.lw[i])
                s.update(b.rd[i])
        for b in new:
            for i in range(b.ncell):
                b.rd[i].extend(s)

    def schedule(self):
        import heapq
        ops = self.ops
        n = len(ops)
        succ = [[] for _ in range(n)]
        indeg = [0] * n
        for o in ops:
            for j in o.deps:
                succ[j].append(o.idx)
                indeg[o.idx] += 1
        ready_t = [0.0] * n
        fin = [0.0] * n
        heaps = {e: [] for e in ENGS}
        free = {e: 0.0 for e in ENGS}
        for o in ops:
            if indeg[o.idx] == 0:
                heapq.heappush(heaps[o.eng], o.idx)
        out = []
        LAT = 0.2
        WIN = 128
        while len(out) < n:
            best = None
            for e in ENGS:
                h = heaps[e]
                if not h:
                    continue
                cands = heapq.nsmallest(WIN, h)
                bi, bs = None, None
                for i in cands:
                    st = max(ready_t[i], free[e])
                    if bs is None or st < bs - 1e-9:
                        bi, bs = i, st
                if best is None or bs < best[0] - 1e-9 or (abs(bs - best[0]) <= 1e-9 and bi < best[1]):
                    best = (bs, bi, e)
            bs, bi, e = best
            heaps[e].remove(bi)
            heapq.heapify(heaps[e])
            o = ops[bi]
            f = bs + o.cost
            free[e] = bs + (0.15 if o.dma else o.cost)
            fin[bi] = f
            out.append(o)
            for k in succ[bi]:
                ready_t[k] = max(ready_t[k], f + LAT)
                indeg[k] -= 1
                if indeg[k] == 0:
                    heapq.heappush(heaps[ops[k].eng], k)
        self.sim_time = max(fin) if fin else 0.0
        return out

    def emit(self):
        nc, es, ops = self.nc, self.es, self.ops
        sched = self.schedule() if SCHEDULE else None
        for o in ops:
            need = []
            for j, raw in o.deps.items():
                p = ops[j]
                if p.dma:
                    need.append(j)
                    continue
                if p.eng == o.eng and not o.dma:
                    if o.eng == "pe" or (not raw and o.eng in RELAX):
                        continue
                need.append(j)
            o.deps = need
            for j in need:
                if not ops[j].dma:
                    ops[j].sig = True
        for o in (sched if sched is not None else ops):
            if o.sig and not o.dma:
                self.nsig[o.eng] += 1
                o.signo = self.nsig[o.eng]
        esem = {e: es.enter_context(nc.semaphore("s_" + e)) for e in ENGS}
        for g in self.dgs:
            g.sem = es.enter_context(nc.semaphore("d_" + g.name))
        streams = {e: [] for e in ENGS}
        for o in (sched if sched is not None else ops):
            streams[o.eng].append(o)
        block = es.enter_context(nc.Block())
        prog = self

        def run(eng_name, engine):
            seen = {e: 0 for e in ENGS}
            dseen = {}
            for o in streams[eng_name]:
                wl, dl = {}, {}
                for j in o.deps:
                    p = ops[j]
                    if p.dma:
                        k = id(p.dg)
                        if dseen.get(k, 0) < p.dval and (k not in dl or dl[k][1] < p.dval):
                            dl[k] = (p.dg, p.dval)
                    elif seen[p.eng] < p.signo:
                        wl[p.eng] = max(wl.get(p.eng, 0), p.signo)
                for e, v in wl.items():
                    engine.wait_ge(esem[e], v)
                    seen[e] = v
                for k, (g, v) in dl.items():
                    engine.wait_ge(g.sem, v)
                    dseen[k] = v
                ins = o.fn(engine)
                if o.dma:
                    ins.then_inc(o.dg.sem, 16)
                elif o.sig:
                    ins.then_inc(esem[eng_name], 1)
            if eng_name == "sp":
                for e in ENGS:
                    if prog.nsig[e]:
                        engine.wait_ge(esem[e], prog.nsig[e])
                for g in prog.dgs:
                    engine.wait_ge(g.sem, g.count)

        @block.tensor
        def _(e):
            run("pe", e)

        @block.scalar
        def _(e):
            run("act", e)

        @block.vector
        def _(e):
            run("dve", e)

        @block.gpsimd
        def _(e):
            run("pool", e)

        @block.sync
        def _(e):
            run("sp", e)


C_ID, C_M01, C_NEG, C_ONE, C_BLK, C_RST, C_SEL, C_O16 = 0, 128, 256, 384, 512, 528, 1040, 1552
NC_CST = 1552 + 512

_off = {}
_o = 0
for _n, _w in (("mcw", 32), ("mcb", 8), ("scw", 64), ("scb", 16), ("fcw", 132), ("fcb", 44),
               ("ln1g", 8), ("ln1b", 8), ("ln2g", 8), ("ln2b", 8), ("mng", 8), ("sng", 8),
               ("ib", 1), ("fb", 1), ("dtb", 1), ("alog", 1), ("inlg", 8), ("inlb", 8)):
    _off[_n] = _o
    _o += _w
NPRM = _o

Q0, K0, V0, O0, I0, F0, Z0, X0, DT0, GA0, GB0 = 0, 512, 1024, 2048, 3072, 3080, 3088, 4112, 6160, 6176, 7200


def _slab(wcols):
    n = wcols.shape[1] // 512
    a = wcols.reshape(KT, 128, n, 512).transpose(2, 1, 0, 3)
    return np.ascontiguousarray(a).reshape(n, 128, KT * 512)


def _pt(v, ntile):
    return np.ascontiguousarray(v.reshape(ntile, 128).T)


def _make_cst():
    c = np.zeros((128, NC_CST), np.float32)
    c[:, C_ID:C_ID + 128] = np.eye(128, dtype=np.float32)
    s = np.arange(128)[:, None]
    t = np.arange(128)[None, :]
    c[:, C_M01:C_M01 + 128] = (s <= t).astype(np.float32)
    c[:, C_NEG:C_NEG + 128] = np.where(s <= t, 0.0, -30000.0).astype(np.float32)
    c[:, C_ONE:C_ONE + 128] = 1.0
    c[0:16, C_BLK:C_BLK + 16] = np.eye(16, dtype=np.float32)
    r = np.ones((16, 512), np.float32)
    r[:, 0::128] = 0.0
    c[0:16, C_RST:C_RST + 512] = r
    sel = np.zeros((8, 4, 128), np.float32)
    for hp in range(4):
        sel[2 * hp, hp, 0:64] = 1.0
        sel[2 * hp + 1, hp, 64:128] = 1.0
    c[0:8, C_SEL:C_SEL + 512] = sel.reshape(8, 512)
    c[0:16, C_O16:C_O16 + 512] = 1.0
    return c


def _layer_arrays(inp, l):
    f32 = np.float32
    w_in = inp["w_in"][l]
    out = {}
    cols = [w_in[:, Q0:Q0 + 1024], w_in[:, X0:X0 + 2048], w_in[:, GA0:GA0 + 1024], w_in[:, GB0:GB0 + 1024],
            w_in[:, V0:V0 + 1024], w_in[:, O0:O0 + 1024], w_in[:, Z0:Z0 + 1024]]
    out["win"] = _slab(np.concatenate(cols, axis=1))
    mini = np.concatenate([w_in[:, I0:I0 + 8], w_in[:, F0:F0 + 8], w_in[:, DT0:DT0 + 16]], axis=1)
    out["winm"] = np.ascontiguousarray(mini.reshape(KT, 128, 32).transpose(1, 0, 2)).reshape(128, 256)
    out["pa"] = _slab(inp["p_a"][l])
    out["pb"] = _slab(inp["p_b"][l])
    out["wo"] = _slab(inp["w_out"][l])
    w_up = inp["w_up"][l]
    perm = []
    for j in range(NJ):
        m = 128 if j < 21 else 64
        perm += list(range(128 * j, 128 * j + m)) + list(range(DFF + 128 * j, DFF + 128 * j + m))
    wup_p = np.zeros((1024, 11 * 512), f32)
    wup_p[:, :len(perm)] = w_up[:, perm]
    out["wup"] = _slab(wup_p)
    wd = np.zeros((NJ * 128, 1024), f32)
    wd[:DFF] = inp["w_down"][l]
    out["wdn"] = np.ascontiguousarray(wd.reshape(NJ, 128, 8, 128).transpose(2, 1, 0, 3)).reshape(8, 128, NJ * 128)
    prm = np.zeros((128, NPRM), f32)

    def put(name, arr):
        a = np.asarray(arr, f32)
        prm[:a.shape[0], _off[name]:_off[name] + a.shape[1]] = a

    mcw = inp["m_conv_w"][l]
    put("mcw", mcw.T.reshape(8, 128, 4).transpose(1, 0, 2).reshape(128, 32))
    put("mcb", _pt(inp["m_conv_b"][l], 8))
    scw = inp["s_conv_w"][l]
    put("scw", scw.T.reshape(16, 128, 4).transpose(1, 0, 2).reshape(128, 64))
    put("scb", _pt(inp["s_conv_b"][l], 16))
    fcw = inp["f_conv_w"][l]
    fcb = inp["f_conv_b"][l]
    fw = np.zeros((128, NJ, 2, 3), f32)
    fb = np.zeros((128, NJ, 2), f32)
    for j in range(NJ):
        m = 128 if j < 21 else 64
        fw[:m, j, 0, :] = fcw[:, 128 * j:128 * j + m].T
        fw[:m, j, 1, :] = fcw[:, DFF + 128 * j:DFF + 128 * j + m].T
        fb[:m, j, 0] = fcb[128 * j:128 * j + m]
        fb[:m, j, 1] = fcb[DFF + 128 * j:DFF + 128 * j + m]
    put("fcw", fw.reshape(128, 132))
    put("fcb", fb.reshape(128, 44))
    put("ln1g", _pt(inp["ln1_g"][l], 8))
    put("ln1b", _pt(inp["ln1_b"][l], 8))
    put("ln2g", _pt(inp["ln2_g"][l], 8))
    put("ln2b", _pt(inp["ln2_b"][l], 8))
    put("mng", _pt(inp["m_norm_g"][l], 8))
    put("sng", _pt(inp["s_norm_g"][l], 8))
    put("ib", inp["m_i_bias"][l].reshape(8, 1))
    put("fb", inp["m_f_bias"][l].reshape(8, 1))
    put("dtb", inp["s_dt_bias"][l].reshape(16, 1))
    put("alog", inp["s_a_log"][l].reshape(16, 1))
    put("inlg", _pt(inp["in_ln_g"], 8))
    put("inlb", _pt(inp["in_ln_b"], 8))
    out["prm"] = prm
    out["dexp"] = np.ascontiguousarray(np.repeat(inp["s_d"][l], S_P).reshape(1, 1024).astype(f32))
    return out


WSHAPES = (("win", [16, 128, 4096]), ("winm", [128, 256]), ("pa", [2, 128, 4096]), ("pb", [2, 128, 4096]),
           ("wo", [2, 128, 4096]), ("wup", [11, 128, 4096]), ("wdn", [8, 128, NJ * 128]))


def build_program(n_units, seq_starts, nlayers, entry_ln, dbg=None, stop=None):
    nc = bass.Bass("TRN2", target_bir_lowering=False)
    es = ExitStack()
    P = Prog(nc, es)
    dbg = dbg or {}
    dbg_out = {}

    def din(name, shape):
        return nc.dram_tensor(name, list(shape), F32, kind="ExternalInput").ap()

    xT_d = din("xT", [n_units, 128, KT * TB])
    oT_d = nc.dram_tensor("oT", [n_units, 128, KT * TB], F32, kind="ExternalOutput").ap()
    cst_d = din("cst", [128, NC_CST])
    Wd, Sd = [], []
    for li in range(nlayers):
        w = {n: din(f"{n}{li}", sh) for n, sh in WSHAPES}
        w["prm"] = din(f"prm{li}", [128, NPRM])
        w["dexp"] = din(f"dexp{li}", [1, 1024])
        Wd.append(w)
        s = {}
        for n, sh in WSHAPES:
            ncell = sh[0] if len(sh) == 3 else 1
            s[n] = Buf(nc.dram_tensor(f"s_{n}{li}", list(sh), BF16).ap(), f"s_{n}{li}", ncell)
        Sd.append(s)

    xT = P.sbuf("xT", [128, KT, TB], F32, KT)
    xTb = P.sbuf("xTb", [128, KT, TB], BF16, KT)
    NWS = 3
    WS = [P.sbuf(f"ws{i}", [128, KT, 512], BF16) for i in range(NWS)]
    ar1 = es.enter_context(nc.sbuf_tensor("ar1", [128, 12288], BF16))
    ar2 = es.enter_context(nc.sbuf_tensor("ar2", [128, 11264], BF16))
    xbcT = Buf(ar1[:, 0:8192].rearrange("p (k t) -> p k t", k=16), "xbcT", 16)
    qkT = Buf(ar1[:, 8192:12288].rearrange("p (k t) -> p k t", k=8), "qkT", 8)
    hidT = Buf(ar1[:, 0:11264].rearrange("p (k t) -> p k t", k=NJ), "hidT", NJ)
    stg = [Buf(ar1[:, 0:8192].bitcast(F32), "stg0"), Buf(ar2[:, 0:8192].bitcast(F32), "stg1")]
    xin = Buf(ar1[:, 0:8192].bitcast(F32).rearrange("p (k t) -> p k t", k=KT), "xin", KT)
    WDb = [Buf(ar2[:, i * 2816:(i + 1) * 2816].rearrange("p (j c) -> p j c", j=NJ), f"wd{i}") for i in range(4)]
    v_aug = Buf(ar2[:, 0:4160].rearrange("p (t h c) -> p t h c", t=NTT, h=M_H), "v_aug", NTT)
    sig_o = Buf(ar2[:, 4160:8256].rearrange("p (t c) -> p t c", t=NTT), "sig_o", NTT)
    silu_z = P.sbuf("silu_z", [128, NTT, 1024], BF16, NTT)
    h_aT = P.sbuf("h_aT", [128, KT, TB], BF16, KT)
    h_bT = P.sbuf("h_bT", [128, KT, TB], BF16, KT)
    mergedT = Buf(silu_z.h[:].rearrange("p t c -> p (t c)").rearrange("p (k t) -> p k t", k=KT), "mergedT", KT)
    T4 = [P.sbuf(f"t4_{i}", [128, 1040], F32) for i in range(4)]
    cst = P.sbuf("cst", [128, NC_CST], F32)
    ident_b = P.sbuf("ident_b", [128, 128], BF16)
    ones_b = P.sbuf("ones_b", [128, 128], BF16)
    negm4 = P.sbuf("negm4", [128, 512], BF16)
    mask_m = P.sbuf("mask_m", [128, 128], F32)
    prm = [P.sbuf(f"prm{li}", [128, NPRM], F32) for li in range(nlayers)]
    nfb = [P.sbuf(f"nfb{li}", [8, 1], F32) for li in range(nlayers)]
    negA = [P.sbuf(f"negA{li}", [16, 1], F32) for li in range(nlayers)]
    halo_qk = [P.sbuf(f"hqk{li}", [128, 8, 3], F32, 8) for li in range(nlayers)]
    halo_x = [P.sbuf(f"hx{li}", [128, 16, 3], F32, 16) for li in range(nlayers)]
    halo_f = [P.sbuf(f"hf{li}", [128, 2 * NJ, 2], F32, 2 * NJ) for li in range(nlayers)]
    Bc = [P.sbuf(f"bc{li}", [8, 1], F32) for li in range(nlayers)]
    Rc = [P.sbuf(f"rc{li}", [8, 1], F32) for li in range(nlayers)]
    Sst = [P.sbuf(f"sst{li}", [128, 4, 130], F32) for li in range(nlayers)]
    Hst = [P.sbuf(f"hst{li}", [128, 16, 64], F32) for li in range(nlayers)]
    Hb = [P.sbuf(f"hb{li}", [128, 16, 64], BF16) for li in range(nlayers)]
    Sb = P.sbuf("sb", [128, 4, 130], BF16)
    G = [P.sbuf(f"g{i}", [16, 512], F32) for i in range(4)]
    Gdt = P.sbuf("gdt", [16, 512], F32)
    sm_cm = P.sbuf("sm_cm", [16, 4], F32)
    sm_R = P.sbuf("sm_R", [16, 4], F32)
    sm_Rp = P.sbuf("sm_Rp", [16, 4], F32)
    sm_gm = P.sbuf("sm_gm", [16, 4], F32)
    sm_cd = P.sbuf("sm_cd", [16, 4], F32)
    sm_dg = P.sbuf("sm_dg", [16, 4, 16], F32)
    gtok = P.sbuf("gtok", [128, NTT, 16], F32)
    gam_bc = P.sbuf("gam_bc", [128, 4, 4], F32)
    stok = P.sbuf("stok", [128, NTT, 64], F32)
    cd_bc = P.sbuf("cd_bc", [128, 4, 16], F32)
    k_tok = P.sbuf("k_tok", [128, 512], BF16)
    qz1 = P.sbuf("qz1", [128, 4, 512], BF16, 4)
    vw = P.sbuf("vw", [128, 8, 130], BF16)
    PT = [P.sbuf(f"pt{i}", [128, 2, 128], BF16) for i in range(2)]
    sm_d = P.sbuf("sm_d", [128, 8], F32)
    sm_ss = P.sbuf("sm_ss", [128, 8], F32)
    sm_ss2 = P.sbuf("sm_ss2", [128, 8], F32)
    junk = P.sbuf("junk", [128, 128], BF16)
    junk2 = P.sbuf("junk2", [128, 256], BF16)
    hgb = Buf(ar2[:, 8256:9280], "hgb")
    ynb = Buf(ar2[:, 9280:10304], "ynb")
    Dm = [P.sbuf(f"dm{li}", [128, 16, 64], BF16) for li in range(nlayers)]
    xs_tok = P.sbuf("xs_tok", [128, 1024], BF16)
    B_tok = P.sbuf("B_tok", [128, 512], BF16)
    xdt = P.sbuf("xdt", [128, 16, 64], BF16)
    xdte = P.sbuf("xdte", [128, 16, 64], BF16)
    CBm = P.sbuf("CBm", [128, 4, 128], F32)
    Zg = [P.sbuf(f"zg{i}", [128, 4, 128], F32) for i in range(2)]
    decb = [P.sbuf(f"dec{i}", [128, 4, 128], BF16) for i in range(2)]
    Gg = [P.sbuf(f"gg{i}", [128, 4, 128], BF16) for i in range(2)]
    PB = [P.psum(f"pb{i}", [128, 512], F32) for i in range(7)]
    PBT = P.psum("pbt", [128, 1024], BF16)

    ident_f = cst[:, C_ID:C_ID + 128]
    mask01 = cst[:, C_M01:C_M01 + 128]
    ones_f = cst[:, C_ONE:C_ONE + 128]

    def fsz(ap):
        n = 1
        for d_ in ap.shape[1:]:
            n *= d_
        return n

    def act(out, in_, func, reads, writes, bias=None, scale=None):
        kw = {}
        if bias is not None:
            kw["bias"] = bias
        if scale is not None:
            kw["scale"] = scale
        P.op("act", lambda e: e.activation(out=out, in_=in_, func=func, **kw), reads, writes,
             cost=0.22 + fsz(out) / 1400.0)

    def ecost(eng, out, mult=1.0):
        if eng == "pool":
            return 0.4 + mult * fsz(out) / 520.0
        if eng == "act":
            return 0.22 + mult * fsz(out) / 1400.0
        return 0.12 + mult * fsz(out) / 960.0

    def tt(eng, out, in0, in1, op, reads, writes):
        P.op(eng, lambda e: e.tensor_tensor(out=out, in0=in0, in1=in1, op=op), reads, writes, cost=ecost(eng, out))

    def ts(eng, out, in0, s1, op0, reads, writes, s2=None, op1=None):
        if op1 is None:
            P.op(eng, lambda e: e.tensor_scalar(out=out, in0=in0, scalar1=s1, scalar2=None, op0=op0), reads, writes,
                 cost=ecost(eng, out))
        else:
            P.op(eng, lambda e: e.tensor_scalar(out=out, in0=in0, scalar1=s1, scalar2=s2, op0=op0, op1=op1),
                 reads, writes, cost=ecost(eng, out))

    def stt(out, in0, scalar, in1, op0, op1, reads, writes):
        P.op("dve", lambda e: e.scalar_tensor_tensor(out=out, in0=in0, scalar=scalar, in1=in1, op0=op0, op1=op1),
             reads, writes, cost=ecost("dve", out))

    def cp(eng, out, in_, reads, writes):
        if eng == "act":
            P.op("act", lambda e: e.copy(out=out, in_=in_), reads, writes, cost=ecost("act", out))
        else:
            P.op(eng, lambda e: e.tensor_copy(out=out, in_=in_), reads, writes, cost=ecost(eng, out))

    def mm(out, lhsT, rhs, start, stop, reads, writes):
        nn = max(fsz(rhs), 64) / 2000.0
        if rhs.dtype == F32:
            nn *= 4
        P.op("pe", lambda e: e.matmul(out, lhsT=lhsT, rhs=rhs, start=start, stop=stop), reads, writes,
             cost=nn + 0.03)

    def tr(out, in_, ident, reads, writes):
        P.op("pe", lambda e: e.transpose(out, in_, ident), reads, writes, cost=0.09)

    def memset(eng, ap, val, writes):
        P.op(eng, lambda e: e.memset(ap, val), (), writes, cost=ecost(eng, ap))

    def recip(out, in_, reads, writes):
        P.op("dve", lambda e: e.reciprocal(out=out, in_=in_), reads, writes, cost=ecost("dve", out, 8.0))

    P.dma("sp", cst[:], cst_d, writes=[cst])
    for li in range(nlayers):
        P.dma("sp", prm[li][:], Wd[li]["prm"], writes=[prm[li]])
    cp("dve", ident_b[:], ident_f, [cst], [ident_b])
    cp("dve", ones_b[:], ones_f, [cst], [ones_b])
    for q in range(4):
        cp("dve", negm4[:, q * 128:(q + 1) * 128], cst[:, C_NEG:C_NEG + 128], [cst], [negm4])
    ts("dve", mask_m[:], mask01, DQK ** -0.5, ALU.mult, [cst], [mask_m])
    for li in range(nlayers):
        o = _off
        ts("dve", nfb[li][:], prm[li][0:8, o["fb"]:o["fb"] + 1], -1.0, ALU.mult, [prm[li]], [nfb[li]])
        act(negA[li][:], prm[li][0:16, o["alog"]:o["alog"] + 1], AF.Exp, [prm[li]], [negA[li]])
        ts("dve", negA[li][:], negA[li][:], -1.0, ALU.mult, [negA[li]], [negA[li]])
    for li in range(nlayers):
        dexp1 = T4[3]
        P.dma("sp", dexp1[:, 0:1024], Wd[li]["dexp"].partition_broadcast(128), writes=[dexp1])
        for h in range(16):
            e2 = h % 2
            ts("dve", Dm[li][:, h, :], cst[:, C_ID + 64 * e2:C_ID + 64 * e2 + 64], dexp1[:, h * 64:h * 64 + 1],
               ALU.mult, [cst, dexp1], [Dm[li]])
    memset("pool", vw[:], 0.0, [vw])
    for i in range(4):
        memset("pool", T4[i][:], 0.0, [T4[i]])
    for i in range(4):
        memset("pool", G[i][:], 0.0, [G[i]])
    for i in range(2):
        memset("pool", Zg[i][:], 0.0, [Zg[i]])
    memset("pool", qz1[:], 0.0, [qz1])

    jobs = []
    for li in range(nlayers):
        for n, sh in WSHAPES:
            if n == "winm":
                jobs.append((Wd[li][n], Sd[li][n].h, [Sd[li][n]], 256, None, li))
            elif n == "wdn":
                for s in range(sh[0]):
                    jobs.append((Wd[li][n][s], Sd[li][n].h[s], [Sd[li][n].c(s)], 2816, None, li))
            else:
                for s in range(sh[0]):
                    sc = {"pa": "mng", "pb": "sng"}.get(n)
                    jobs.append((Wd[li][n][s], Sd[li][n].h[s], [Sd[li][n].c(s)], 4096, sc, li))
    cast_engs = ("act", "dve", "act")

    def pre_load(ji):
        src, dst, cells, F, sc, li = jobs[ji]
        st = stg[ji % 2]
        P.dma("sp", st[:, 0:F], src, writes=[st])

    def pre_cast_store(ji):
        src, dst, cells, F, sc, li = jobs[ji]
        st = stg[ji % 2]
        wb = WS[ji % NWS]
        wflat = wb[:].rearrange("p k c -> p (k c)")
        if sc is not None:
            g = prm[li][:, _off[sc]:_off[sc] + 8]
            tt("dve", wb[:], st[:, 0:4096].rearrange("p (k c) -> p k c", k=KT),
               g.unsqueeze(2).to_broadcast([128, KT, 512]), ALU.mult, [st, prm[li]], [wb])
        else:
            cp(cast_engs[ji % 3], wflat[:, 0:F], st[:, 0:F], [st], [wb])
        P.dma("sp", dst, wflat[:, 0:F], reads=[wb], writes=cells, sem_buf=wb)

    for ji in range(len(jobs) + 1):
        if ji < len(jobs):
            pre_load(ji)
        if ji >= 1:
            pre_cast_store(ji - 1)
    P.fence(stg, [xbcT, qkT, hidT, xin] + WDb + [v_aug, sig_o, hgb, ynb])

    ws_ctr = [0]

    def load_slab(li, name, s):
        wb = WS[ws_ctr[0] % NWS]
        ws_ctr[0] += 1
        sb = Sd[li][name]
        P.dma("sp", wb[:].rearrange("p k c -> p (k c)"), sb.h[s], reads=[sb.c(s)], writes=[wb])
        return wb

    gb_ctr = [0]

    def gbank():
        b = PB[gb_ctr[0] % 6]
        gb_ctr[0] += 1
        return b

    def layernorm(g_ap, b_ap, pbuf, src=None):
        src = xT if src is None else src
        ps_m, ps_q = PB[5], PB[6]
        for kt in range(KT):
            tb = T4[1 + 2 * (kt % 2)]
            tbv = tb[:].bitcast(BF16)
            act(tbv[:, 0:512], src[:, kt, :], AF.Copy, [src.c(kt)], [tb])
            act(tbv[:, 512:1024], src[:, kt, :], AF.Square, [src.c(kt)], [tb])
            mm(ps_m[:, :], ones_b[:], tbv[:, 0:512], kt == 0, kt == KT - 1, [ones_b, tb], [ps_m])
            mm(ps_q[:, :], ones_b[:], tbv[:, 512:1024], kt == 0, kt == KT - 1, [ones_b, tb], [ps_q])
        mean, var = T4[0], T4[2]
        act(mean[:, 0:512], ps_m[:, :], AF.Copy, [ps_m], [mean], scale=1.0 / D)
        tt("dve", var[:, 0:512], mean[:, 0:512], mean[:, 0:512], ALU.mult, [mean], [var])
        stt(var[:, 0:512], ps_q[:, :], 1.0 / D, var[:, 0:512], ALU.mult, ALU.subtract, [ps_q, var], [var])
        ts("dve", var[:, 0:512], var[:, 0:512], LN_EPS, ALU.add, [var], [var])
        act(var[:, 0:512], var[:, 0:512], AF.Ln, [var], [var])
        act(var[:, 0:512], var[:, 0:512], AF.Exp, [var], [var], scale=-0.5)
        for kt in range(KT):
            tmp = T4[1 + 2 * (kt % 2)]
            tt("pool", tmp[:, 0:512], src[:, kt, :], mean[:, 0:512], ALU.subtract, [src.c(kt), mean], [tmp])
            tt("dve", tmp[:, 0:512], tmp[:, 0:512], var[:, 0:512], ALU.mult, [tmp, var], [tmp])
            act(xT[:, kt, :], tmp[:, 0:512], AF.Identity, [tmp, pbuf], [xT.c(kt)],
                bias=b_ap[:, kt:kt + 1], scale=g_ap[:, kt:kt + 1])
            cp("dve", xTb[:, kt, :], xT[:, kt, :], [xT.c(kt)], [xTb.c(kt)])

    acc_ctr = [0]

    def conv_pair(items, pbuf):
        accs = []
        for i, (ps, M, Kc, halo_buf, hcell, w_ap, b_ap) in enumerate(items):
            acc = T4[acc_ctr[0] % 4]
            acc_ctr[0] += 1
            Hh = Kc - 1
            act(acc[0:M, 0:512], ps[0:M, :], AF.Identity, [ps, pbuf], [acc],
                bias=b_ap[0:M, 0:1], scale=w_ap[0:M, Hh:Hh + 1])
            accs.append(acc)
        Kc = items[0][2]
        Hh = Kc - 1
        for k in range(Kc - 1):
            sh = Hh - k
            for i, (ps, M, _k, halo_buf, hcell, w_ap, b_ap) in enumerate(items):
                acc = accs[i]
                stt(acc[0:M, sh:512], ps[0:M, 0:512 - sh], w_ap[0:M, k:k + 1], acc[0:M, sh:512], ALU.mult, ALU.add,
                    [ps, pbuf, acc], [acc])
                stt(acc[0:M, 0:sh], halo_buf[0:M, hcell, k:Hh], w_ap[0:M, k:k + 1], acc[0:M, 0:sh], ALU.mult,
                    ALU.add, [halo_buf.c(hcell), pbuf, acc], [acc])
        for i, (ps, M, Kc_, halo_buf, hcell, w_ap, b_ap) in enumerate(items):
            cp("act", halo_buf[0:M, hcell, 0:Hh], ps[0:M, 512 - Hh:512], [ps], [halo_buf.c(hcell)])
        return accs

    def gemm_A(wb, c0, M, rhsbuf, ps):
        for kt in range(KT):
            mm(ps[0:M, :], wb[:, kt, c0:c0 + M], rhsbuf[:, kt, :], kt == 0, kt == KT - 1,
               [wb, rhsbuf.c(kt)], [ps])

    def mlstm_gates(li, ps_i, ps_f):
        o = _off
        pl = prm[li]
        act(G[0][0:8, :], ps_f[0:8, :], AF.Exp, [ps_f, nfb[li]], [G[0]], bias=nfb[li][:], scale=-1.0)
        act(G[0][0:8, :], G[0][0:8, :], AF.Ln, [G[0]], [G[0]], bias=1.0)
        P.op("dve", lambda e: e.tensor_tensor_scan(out=G[1][0:8, :], data0=cst[0:8, C_O16:C_O16 + 512],
                                                   data1=G[0][0:8, :], initial=Bc[li][:],
                                                   op0=ALU.mult, op1=ALU.subtract),
             [cst, G[0], Bc[li]], [G[1]])
        cp("pool", Bc[li][:], G[1][0:8, 511:512], [G[1]], [Bc[li]])
        yield
        stt(G[2][0:8, :], ps_i[0:8, :], pl[0:8, o["ib"]:o["ib"] + 1], G[1][0:8, :], ALU.add, ALU.subtract,
            [ps_i, pl, G[1]], [G[2]])
        P.op("dve", lambda e: e.tensor_reduce(out=sm_cm[0:8, :], in_=G[2][0:8, :].rearrange("p (c t) -> p c t", c=4),
                                              axis=AX.X, op=ALU.max), [G[2]], [sm_cm])
        P.op("dve", lambda e: e.tensor_tensor_scan(out=sm_R[0:8, :], data0=sm_cm[0:8, :], data1=sm_cm[0:8, :],
                                                   initial=Rc[li][:], op0=ALU.max, op1=ALU.max),
             [sm_cm, Rc[li]], [sm_R])
        cp("pool", sm_Rp[0:8, 0:1], Rc[li][:], [Rc[li]], [sm_Rp])
        cp("pool", sm_Rp[0:8, 1:4], sm_R[0:8, 0:3], [sm_R], [sm_Rp])
        cp("pool", Rc[li][:], sm_R[0:8, 3:4], [sm_R], [Rc[li]])
        yield
        tt("dve", sm_gm[0:8, :], sm_Rp[0:8, :], sm_R[0:8, :], ALU.subtract, [sm_Rp, sm_R], [sm_gm])
        act(sm_gm[0:8, :], sm_gm[0:8, :], AF.Exp, [sm_gm], [sm_gm])
        Rb = sm_R[0:8, :].unsqueeze(2).to_broadcast([8, 4, 128])
        g2v = G[2][0:8, :].rearrange("p (c t) -> p c t", c=4)
        g1v = G[1][0:8, :].rearrange("p (c t) -> p c t", c=4)
        g3v = G[3][0:8, :].rearrange("p (c t) -> p c t", c=4)
        tt("dve", g2v, g2v, Rb, ALU.subtract, [G[2], sm_R], [G[2]])
        act(G[2][0:8, :], G[2][0:8, :], AF.Exp, [G[2]], [G[2]])
        yield
        tt("dve", g3v, g1v, Rb, ALU.add, [G[1], sm_R], [G[3]])
        act(G[3][0:8, :], G[3][0:8, :], AF.Exp, [G[3]], [G[3]], scale=-1.0)
        yield
        ps_g = PB[2]
        for t4 in range(NTT):
            tsl = slice(t4 * 128, (t4 + 1) * 128)
            tr(ps_g[:, t4 * 16:t4 * 16 + 8], G[2][0:8, tsl], cst[0:8, C_ID:C_ID + 8], [G[2], cst], [ps_g])
            tr(ps_g[:, t4 * 16 + 8:t4 * 16 + 16], G[3][0:8, tsl], cst[0:8, C_ID:C_ID + 8], [G[3], cst], [ps_g])
        for hp in range(4):
            mm(ps_g[:, 64 + hp * 4:64 + hp * 4 + 4], cst[0:8, C_SEL + hp * 128:C_SEL + (hp + 1) * 128],
               sm_gm[0:8, :], True, True, [cst, sm_gm], [ps_g])
        cp("act", gtok[:].rearrange("p t c -> p (t c)"), ps_g[:, 0:64], [ps_g], [gtok])
        cp("act", gam_bc[:].rearrange("p h c -> p (h c)"), ps_g[:, 64:80], [ps_g], [gam_bc])
        yield

    import os as _os
    mstop = int(_os.environ.get("MSTOP", "0"))

    def mchk(k):
        if mstop == k:
            raise _Stop()

    def mlstm_chunk(li, c):
        tsl = slice(c * 128, (c + 1) * 128)
        S = Sst[li]
        for hp in range(4):
            tr(PBT[:, hp * 128:(hp + 1) * 128], qkT[:, 4 + hp, tsl], ident_b[:], [qkT.c(4 + hp), ident_b], [PBT])
        cp("act", k_tok[:], PBT[:, 0:512], [PBT], [k_tok])
        mchk(1)
        yield
        tt("pool", vw[:, :, 0:129], v_aug[:, c, :, 0:129],
           gtok[:, c, 0:8].unsqueeze(2).to_broadcast([128, 8, 129]), ALU.mult, [v_aug.c(c), gtok], [vw])
        tt("pool", S[:, :, 0:129], S[:, :, 0:129],
           gam_bc[:, :, c:c + 1].to_broadcast([128, 4, 129]), ALU.mult, [S, gam_bc], [S])
        act(Sb[:, :, 0:129], S[:, :, 0:129], AF.Copy, [S], [Sb], scale=DQK ** -0.5)
        osb = T4[0][:, 0:1040].rearrange("p (h c) -> p h c", h=8)
        mchk(2)
        yield
        for hp in range(4):
            X, Y, Ub = PB[0], PB[1], PB[2]
            pt = PT[hp % 2]
            QZ = (qkT, qz1)
            for e2 in range(2):
                mm(X[:, e2 * 128:(e2 + 1) * 128], qkT[:, 4 + hp, tsl], QZ[e2][:, hp, tsl], True, True,
                   [qkT.c(4 + hp), QZ[e2].c(hp)], [X])
            tt("dve", pt[:], X[:, 0:256].rearrange("p (h t) -> p h t", h=2),
               mask_m[:].unsqueeze(1).to_broadcast([128, 2, 128]), ALU.mult, [X, mask_m], [pt])
            mchk(3)
            yield
            for e2 in range(2):
                h = 2 * hp + e2
                mm(Y[:, e2 * 130:e2 * 130 + 129], pt[:, e2, :], vw[:, h, 0:129], True, False, [pt, vw], [Y])
                mm(Y[:, e2 * 130:e2 * 130 + 129], QZ[e2][:, hp, tsl], Sb[:, hp, 0:129], False, True,
                   [QZ[e2].c(hp), Sb], [Y])
            cp("act", T4[0][:, hp * 260:hp * 260 + 260], Y[:, 0:260], [Y], [T4[0]])
            mchk(4)
            yield
            mm(Ub[:, 0:260], k_tok[:, hp * 128:(hp + 1) * 128],
               vw[:, 2 * hp:2 * hp + 2, :].rearrange("p h c -> p (h c)"), True, True, [k_tok, vw], [Ub])
            tt("dve", S[0:64, hp, 0:129], S[0:64, hp, 0:129], Ub[0:64, 0:129], ALU.add, [S, Ub], [S])
            tt("dve", S[64:128, hp, 0:129], S[64:128, hp, 0:129], Ub[64:128, 130:259], ALU.add, [S, Ub], [S])
            mchk(5)
            yield
        mchk(6)

    def mlstm_tail(li, c):
        tsl = slice(c * 128, (c + 1) * 128)
        osb = T4[0][:, 0:1040].rearrange("p (h c) -> p h c", h=8)
        act(sm_d[:], osb[:, :, 128], AF.Abs, [T4[0]], [sm_d])
        tt("dve", sm_d[:], sm_d[:], gtok[:, c, 8:16], ALU.max, [sm_d, gtok], [sm_d])
        recip(sm_d[:], sm_d[:], [sm_d], [sm_d])
        hv = T4[1][:, 0:1024].rearrange("p (h c) -> p h c", h=8)
        tt("dve", hv, osb[:, :, 0:128], sm_d[:].unsqueeze(2).to_broadcast([128, 8, 128]), ALU.mult,
           [T4[0], sm_d], [T4[1]])
        yield
        for h in range(8):
            P.op("act", lambda e, h=h: e.activation(out=junk[:, 0:128], in_=T4[1][:, h * 128:(h + 1) * 128],
                                                    func=AF.Square, accum_out=sm_ss[:, h:h + 1]),
                 [T4[1]], [junk, sm_ss])
            if h % 4 == 3:
                yield
        ts("dve", sm_ss[:], sm_ss[:], 1.0 / DV, ALU.mult, [sm_ss], [sm_ss], s2=RMS_EPS, op1=ALU.add)
        act(sm_ss[:], sm_ss[:], AF.Ln, [sm_ss], [sm_ss])
        act(sm_ss[:], sm_ss[:], AF.Exp, [sm_ss], [sm_ss], scale=-0.5)
        yield
        tt("dve", hv, hv, sm_ss[:].unsqueeze(2).to_broadcast([128, 8, 128]), ALU.mult, [T4[1], sm_ss], [T4[1]])
        tt("pool", hgb[:, :], T4[1][:, 0:1024], sig_o[:, c, :], ALU.mult, [T4[1], sig_o.c(c)], [hgb])
        yield
        for h in range(8):
            tr(PBT[:, h * 128:(h + 1) * 128], hgb[:, h * 128:(h + 1) * 128], ident_b[:], [hgb, ident_b], [PBT])
        cp("act", h_aT[:, :, tsl], PBT[:, :].rearrange("p (h t) -> p h t", h=8), [PBT], [h_aT])
        yield

    def ssd_gates(li):
        Gq = (Gdt, G[1], G[2], G[3])
        ts("dve", G[1][:], Gdt[:], negA[li][:], ALU.mult, [Gdt, negA[li]], [G[1]])
        P.op("dve", lambda e: e.tensor_tensor_scan(out=G[2][:], data0=cst[0:16, C_RST:C_RST + 512], data1=G[1][:],
                                                   initial=0.0, op0=ALU.mult, op1=ALU.add),
             [cst, G[1]], [G[2]])
        act(G[3][:], G[2][:], AF.Exp, [G[2]], [G[3]])
        yield
        g2v = G[2][:].rearrange("p (c t) -> p c t", c=4)
        g1v = G[1][:].rearrange("p (c t) -> p c t", c=4)
        tt("dve", g1v, g2v[:, :, 127:128].to_broadcast([16, 4, 128]), g2v, ALU.subtract, [G[2]], [G[1]])
        act(G[1][:], G[1][:], AF.Exp, [G[1]], [G[1]])
        yield
        act(sm_cd[:], g2v[:, :, 127], AF.Exp, [G[2]], [sm_cd])
        tt("dve", sm_dg[:], cst[0:16, C_BLK:C_BLK + 16].unsqueeze(1).to_broadcast([16, 4, 16]),
           sm_cd[:].unsqueeze(2).to_broadcast([16, 4, 16]), ALU.mult, [cst, sm_cd], [sm_dg])
        ps_g = PB[6]
        for t4 in range(NTT):
            tsl = slice(t4 * 128, (t4 + 1) * 128)
            for q, gi in enumerate((0, 3, 1, 2)):
                tr(ps_g[:, t4 * 64 + q * 16:t4 * 64 + q * 16 + 16], Gq[gi][:, tsl], cst[0:16, C_ID:C_ID + 16],
                   [Gq[gi], cst], [ps_g])
        mm(ps_g[:, 256:320], cst[0:16, C_ONE:C_ONE + 128], sm_dg[:].rearrange("p c h -> p (c h)"), True, True,
           [cst, sm_dg], [ps_g])
        cp("act", stok[:].rearrange("p t c -> p (t c)"), ps_g[:, 0:256], [ps_g], [stok])
        ts("dve", stok[:, :, 48:64], stok[:, :, 48:64], -1.0, ALU.mult, [stok], [stok])
        cp("act", cd_bc[:].rearrange("p c h -> p (c h)"), ps_g[:, 256:320], [ps_g], [cd_bc])
        yield

    def ssd_chunk(li, c):
        tsl = slice(c * 128, (c + 1) * 128)
        H = Hst[li]
        for kt in range(8):
            tr(PBT[:, kt * 128:(kt + 1) * 128], xbcT[:, kt, tsl], ident_b[:], [xbcT.c(kt), ident_b], [PBT])
        cp("act", xs_tok[:], PBT[:, :], [PBT], [xs_tok])
        yield
        for g in range(4):
            tr(PBT[:, g * 128:(g + 1) * 128], xbcT[:, 8 + g, tsl], ident_b[:], [xbcT.c(8 + g), ident_b], [PBT])
        cp("act", B_tok[:], PBT[:, 0:512], [PBT], [B_tok])
        yield
        xsv = xs_tok[:].rearrange("p (h c) -> p h c", h=16)
        tt("pool", xdt[:], xsv, stok[:, c, 0:16].unsqueeze(2).to_broadcast([128, 16, 64]), ALU.mult,
           [xs_tok, stok], [xdt])
        tt("pool", xdte[:], xdt[:], stok[:, c, 32:48].unsqueeze(2).to_broadcast([128, 16, 64]), ALU.mult,
           [xdt, stok], [xdte])
        yield
        for g in range(4):
            mm(PB[3][:, g * 128:(g + 1) * 128], xbcT[:, 8 + g, tsl], xbcT[:, 12 + g, tsl], True, True,
               [xbcT.c(8 + g), xbcT.c(12 + g)], [PB[3]])
        tt("dve", CBm[:], PB[3][:, :].rearrange("p (g t) -> p g t", g=4),
           mask01.unsqueeze(1).to_broadcast([128, 4, 128]), ALU.mult, [PB[3], cst], [CBm])
        yield
        for g in range(4):
            zg, A_, dec, gg, Yb = Zg[g % 2], PB[4], decb[g % 2], Gg[g % 2], PB[5]
            tt("pool", zg[0:16], G[2][:, tsl].unsqueeze(1).to_broadcast([16, 4, 128]),
               cst[0:16, C_BLK + 4 * g:C_BLK + 4 * g + 4].unsqueeze(2).to_broadcast([16, 4, 128]), ALU.mult,
               [G[2], cst], [zg])
            mm(A_[:, :], ones_f, zg[:].rearrange("p h t -> p (h t)"), True, False,
               [cst, zg], [A_])
            mm(A_[:, :], ident_b[:], negm4[:], False, True, [ident_b, negm4], [A_])
            yield
            for hh in range(4):
                h = 4 * g + hh
                act(dec[:, hh, :], A_[:, hh * 128:(hh + 1) * 128], AF.Exp, [A_, stok], [dec],
                    bias=stok[:, c, 48 + h:49 + h])
            tt("dve", gg[:], dec[:], CBm[:, g, :].unsqueeze(1).to_broadcast([128, 4, 128]), ALU.mult,
               [dec, CBm], [gg])
            yield
            for hh in range(4):
                h = 4 * g + hh
                mm(Yb[:, hh * 64:(hh + 1) * 64], gg[:, hh, :], xdt[:, h, :], True, False, [gg, xdt], [Yb])
                mm(Yb[:, hh * 64:(hh + 1) * 64], xbcT[:, h // 2, tsl], Dm[li][:, h, :], False, True,
                   [xbcT.c(h // 2), Dm[li]], [Yb])
            mm(Yb[:, 256:512], xbcT[:, 12 + g, tsl], Hb[li][:, 4 * g:4 * g + 4, :].rearrange("p h c -> p (h c)"),
               True, True, [xbcT.c(12 + g), Hb[li]], [Yb])
            yv = T4[2][:, g * 256:(g + 1) * 256]
            cp("act", yv, Yb[:, 256:512], [Yb], [T4[2]])
            yield
            yv3 = yv.rearrange("p (h c) -> p h c", h=4)
            tt("dve", yv3, yv3, stok[:, c, 16 + 4 * g:16 + 4 * g + 4].unsqueeze(2).to_broadcast([128, 4, 64]),
               ALU.mult, [T4[2], stok], [T4[2]])
            tt("dve", yv, yv, Yb[:, 0:256], ALU.add, [T4[2], Yb], [T4[2]])
            yield
            U = PB[6]
            mm(U[:, 0:256], B_tok[:, g * 128:(g + 1) * 128],
               xdte[:, 4 * g:4 * g + 4, :].rearrange("p h c -> p (h c)"), True, True, [B_tok, xdte], [U])
            hv = H[:, 4 * g:4 * g + 4, :]
            tt("pool", hv, hv, cd_bc[:, c, 4 * g:4 * g + 4].unsqueeze(2).to_broadcast([128, 4, 64]), ALU.mult,
               [H, cd_bc], [H])
            tt("dve", hv, hv, U[:, 0:256].rearrange("p (h c) -> p h c", h=4), ALU.add, [H, U], [H])
            yield
        cp("act", Hb[li][:], H[:], [H], [Hb[li]])
        yield

    def ssd_tail(li, c):
        tsl = slice(c * 128, (c + 1) * 128)
        tt("pool", T4[3][:, 0:1024], T4[2][:, 0:1024], silu_z[:, c, :], ALU.mult, [T4[2], silu_z.c(c)], [T4[3]])
        yield
        for g in range(4):
            P.op("act", lambda e, g=g: e.activation(out=junk2[:, 0:256], in_=T4[3][:, g * 256:(g + 1) * 256],
                                                    func=AF.Square, accum_out=sm_ss2[:, g:g + 1]),
                 [T4[3]], [junk2, sm_ss2])
        yield
        ts("dve", sm_ss2[:, 0:4], sm_ss2[:, 0:4], 1.0 / 256, ALU.mult, [sm_ss2], [sm_ss2], s2=RMS_EPS, op1=ALU.add)
        act(sm_ss2[:, 0:4], sm_ss2[:, 0:4], AF.Ln, [sm_ss2], [sm_ss2])
        act(sm_ss2[:, 0:4], sm_ss2[:, 0:4], AF.Exp, [sm_ss2], [sm_ss2], scale=-0.5)
        yield
        tt("dve", ynb[:, :].rearrange("p (g c) -> p g c", g=4),
           T4[3][:, 0:1024].rearrange("p (g c) -> p g c", g=4),
           sm_ss2[:, 0:4].unsqueeze(2).to_broadcast([128, 4, 256]), ALU.mult, [T4[3], sm_ss2], [ynb])
        yield
        for kt in range(8):
            tr(PBT[:, kt * 128:(kt + 1) * 128], ynb[:, kt * 128:(kt + 1) * 128], ident_b[:], [ynb, ident_b], [PBT])
        cp("act", h_bT[:, :, tsl], PBT[:, :].rearrange("p (h t) -> p h t", h=8), [PBT], [h_bT])
        yield

    dexp_loaded = [False]

    class _Stop(Exception):
        pass

    def chk(name):
        if stop == name:
            raise _Stop()

    def layer(li):
        try:
            layer_(li)
        except _Stop:
            pass

    def layer_(li):
        o = _off
        pl = prm[li]
        chk("start")
        P.fence([hidT], [xbcT, qkT])
        P.fence(WDb, [v_aug, sig_o, hgb, ynb])
        P.fence([mergedT], [silu_z])
        for s in range(2):
            wb = load_slab(li, "win", s)
            for pr in range(2):
                items = []
                for t4 in (2 * pr, 2 * pr + 1):
                    tq = s * 4 + t4
                    ps = gbank()
                    gemm_A(wb, t4 * 128, 128, xTb, ps)
                    items.append((ps, 128, 4, halo_qk[li], tq, pl[:, o["mcw"] + tq * 4:o["mcw"] + tq * 4 + 4],
                                  pl[:, o["mcb"] + tq:o["mcb"] + tq + 1]))
                accs = conv_pair(items, pl)
                for it, acc in zip(items, accs):
                    tq = it[4]
                    if tq < 4:
                        memset("pool", qkT[64:128, tq, :], 0.0, [qkT.c(tq)])
                        act(qkT[0:64, tq, :], acc[0:64, 0:512], AF.Silu, [acc], [qkT.c(tq)])
                        act(qz1[64:128, tq, :], acc[64:128, 0:512], AF.Silu, [acc], [qz1.c(tq)])
                    else:
                        act(qkT[:, tq, :], acc[:, 0:512], AF.Silu, [acc], [qkT.c(tq)])
        chk("qk")
        for s in range(4):
            wb = load_slab(li, "win", 2 + s)
            for pr in range(2):
                items = []
                for t4 in (2 * pr, 2 * pr + 1):
                    tq = s * 4 + t4
                    ps = gbank()
                    gemm_A(wb, t4 * 128, 128, xTb, ps)
                    items.append((ps, 128, 4, halo_x[li], tq, pl[:, o["scw"] + tq * 4:o["scw"] + tq * 4 + 4],
                                  pl[:, o["scb"] + tq:o["scb"] + tq + 1]))
                accs = conv_pair(items, pl)
                for it, acc in zip(items, accs):
                    act(xbcT[:, it[4], :], acc[:, 0:512], AF.Silu, [acc], [xbcT.c(it[4])])
        chk("xbc")
        for which in range(3):
            for s in range(2):
                wb = load_slab(li, "win", 10 + 2 * which + s)
                for t4 in range(NTT):
                    ps = gbank()
                    for kt in range(KT):
                        mm(ps[:, :], xTb[:, kt, t4 * 128:(t4 + 1) * 128], wb[:, kt, :], kt == 0, kt == KT - 1,
                           [xTb.c(kt), wb], [ps])
                    if which == 0:
                        cp("act", v_aug[:, t4, 4 * s:4 * s + 4, 0:128], ps[:, :].rearrange("p (h c) -> p h c", h=4),
                           [ps], [v_aug.c(t4)])
                    elif which == 1:
                        act(sig_o[:, t4, s * 512:(s + 1) * 512], ps[:, :], AF.Sigmoid, [ps], [sig_o.c(t4)])
                    else:
                        act(silu_z[:, t4, s * 512:(s + 1) * 512], ps[:, :], AF.Silu, [ps], [silu_z.c(t4)])
        for t4 in range(NTT):
            memset("pool", v_aug[:, t4, :, 128:129], 1.0, [v_aug.c(t4)])
        chk("voz")
        wb = WS[ws_ctr[0] % NWS]
        ws_ctr[0] += 1
        wbf = wb[:].rearrange("p k c -> p (k c)")
        P.dma("sp", wbf[:, 0:256], Sd[li]["winm"].h, reads=[Sd[li]["winm"]], writes=[wb])
        wm = wbf[:, 0:256].rearrange("p (k c) -> p k c", k=KT)
        ps_i, ps_f, ps_dt = PB[3], PB[4], PB[5]
        for (ps_, c0, M) in ((ps_i, 0, 8), (ps_f, 8, 8), (ps_dt, 16, 16)):
            for kt in range(KT):
                mm(ps_[0:M, :], wm[:, kt, c0:c0 + M], xTb[:, kt, :], kt == 0, kt == KT - 1, [wb, xTb.c(kt)], [ps_])
        act(Gdt[:], ps_dt[0:16, :], AF.Exp, [ps_dt, pl], [Gdt], bias=pl[0:16, o["dtb"]:o["dtb"] + 1])
        act(Gdt[:], Gdt[:], AF.Ln, [Gdt], [Gdt], bias=1.0)
        chk("mini")
        for _ in mlstm_gates(li, ps_i, ps_f):
            pass
        if stop != "mgates":
            for _ in ssd_gates(li):
                pass

        for st_ in range(NTT + 1):
            gens = []
            if st_ < NTT:
                gens.append(mlstm_chunk(li, st_))
                if stop not in ("mgates", "mlstm"):
                    gens.append(ssd_chunk(li, st_))
            if st_ >= 1:
                gens.append(mlstm_tail(li, st_ - 1))
                if stop not in ("mgates", "mlstm"):
                    gens.append(ssd_tail(li, st_ - 1))
            while gens:
                for g_ in list(gens):
                    try:
                        next(g_)
                    except StopIteration:
                        gens.remove(g_)
        chk("ssd")
        if "h_aT" in dbg:
            dump("h_aT", h_aT, li)
            dump("h_bT", h_bT, li)
        P.fence([silu_z], [mergedT])
        for br, (pname, gslab, hT) in enumerate((("pa", 6, h_aT), ("pb", 8, h_bT))):
            for half in range(2):
                wp = load_slab(li, pname, half)
                wg = load_slab(li, "win", gslab + half)
                ps1s = []
                for t4 in range(4):
                    ps1 = PB[t4]
                    gemm_A(wp, t4 * 128, 128, hT, ps1)
                    ps1s.append(ps1)
                for t4 in range(4):
                    kt_o = half * 4 + t4
                    ps1 = ps1s[t4]
                    ps2 = PB[4 + t4 % 2]
                    gemm_A(wg, t4 * 128, 128, xTb, ps2)
                    sgb = T4[kt_o % 4]
                    sg = sgb[:, 0:512]
                    act(sg, ps2[:, :], AF.Sigmoid, [ps2], [sgb])
                    if br == 0:
                        tt("dve", mergedT[:, kt_o, :], ps1[:, :], sg, ALU.mult, [ps1, sgb], [mergedT.c(kt_o)])
                    else:
                        tt("dve", sg, ps1[:, :], sg, ALU.mult, [ps1, sgb], [sgb])
                        tt("pool", mergedT[:, kt_o, :], mergedT[:, kt_o, :], sg, ALU.add,
                           [mergedT.c(kt_o), sgb], [mergedT.c(kt_o)])
        chk("merge")
        for half in range(2):
            wb = load_slab(li, "wo", half)
            for t4 in range(4):
                kt_o = half * 4 + t4
                ps = gbank()
                gemm_A(wb, t4 * 128, 128, mergedT, ps)
                stt(xT[:, kt_o, :], xT[:, kt_o, :], ALPHA, ps[:, :], ALU.mult, ALU.add, [xT.c(kt_o), ps],
                    [xT.c(kt_o)])
        layernorm(pl[:, o["ln1g"]:o["ln1g"] + 8], pl[:, o["ln1b"]:o["ln1b"] + 8], pl)
        if "x1" in dbg:
            dump("x1", xT, li)
        chk("ln1")
        P.fence([xbcT, qkT], [hidT])
        P.fence([v_aug, sig_o, hgb, ynb], WDb)
        for s in range(11):
            wb = load_slab(li, "wup", s)
            for jj in range(2):
                j = 2 * s + jj
                M = 128 if j < 21 else 64
                base = jj * 256
                items = []
                for gv in range(2):
                    ps = gbank()
                    gemm_A(wb, base + gv * M, M, xTb, ps)
                    hc = 2 * j + gv
                    wof = o["fcw"] + hc * 3
                    items.append((ps, M, 3, halo_f[li], hc, pl[:, wof:wof + 3], pl[:, o["fcb"] + hc:o["fcb"] + hc + 1]))
                accs = conv_pair(items, pl)
                act(accs[0][0:M, 0:512], accs[0][0:M, 0:512], AF.Silu, [accs[0]], [accs[0]])
                tt("pool", hidT[0:M, j, :], accs[0][0:M, 0:512], accs[1][0:M, 0:512], ALU.mult, [accs[0], accs[1]],
                   [hidT.c(j)])
        chk("ffnup")
        memset("pool", hidT[64:128, NJ - 1, :], 0.0, [hidT.c(NJ - 1)])
        for q in range(8):
            wd = WDb[q % 4]
            P.dma("sp", wd[:].rearrange("p j c -> p (j c)"), Sd[li]["wdn"].h[q], reads=[Sd[li]["wdn"].c(q)],
                  writes=[wd])
            kt_o = q
            ps = gbank()
            for j in range(NJ):
                mm(ps[:, :], wd[:, j, :], hidT[:, j, :], j == 0, j == NJ - 1, [wd, hidT.c(j)], [ps])
            stt(xT[:, kt_o, :], xT[:, kt_o, :], ALPHA, ps[:, :], ALU.mult, ALU.add, [xT.c(kt_o), ps],
                [xT.c(kt_o)])
        layernorm(pl[:, o["ln2g"]:o["ln2g"] + 8], pl[:, o["ln2b"]:o["ln2b"] + 8], pl)

    def dump(name, buf, li):
        key = f"dbg_{name}{li}"
        if key in dbg_out:
            return
        d = nc.dram_tensor(key, [128, KT * TB], buf.h.dtype if hasattr(buf.h, "dtype") else F32,
                           kind="ExternalOutput").ap()
        dbg_out[key] = d
        P.dma("sp", d, buf[:].rearrange("p k t -> p (k t)"), reads=[buf], sem_buf=buf)

    for u in range(n_units):
        if u in seq_starts:
            for li in range(nlayers):
                memset("pool", halo_qk[li][:], 0.0, [halo_qk[li]])
                memset("pool", halo_x[li][:], 0.0, [halo_x[li]])
                memset("pool", halo_f[li][:], 0.0, [halo_f[li]])
                memset("pool", Bc[li][:], 0.0, [Bc[li]])
                memset("pool", Rc[li][:], NEG_BIG, [Rc[li]])
                memset("pool", Sst[li][:], 0.0, [Sst[li]])
                memset("pool", Hst[li][:], 0.0, [Hst[li]])
                memset("pool", Hb[li][:], 0.0, [Hb[li]])
        P.fence([xbcT, qkT, hidT], [xin])
        P.dma("sp", xin[:].rearrange("p k t -> p (k t)"), xT_d[u], writes=[xin])
        if entry_ln:
            layernorm(prm[0][:, _off["inlg"]:_off["inlg"] + 8], prm[0][:, _off["inlb"]:_off["inlb"] + 8], prm[0],
                      src=xin)
        else:
            for kt in range(KT):
                cp("pool", xT[:, kt, :], xin[:, kt, :], [xin.c(kt)], [xT.c(kt)])
                cp("act", xTb[:, kt, :], xin[:, kt, :], [xin.c(kt)], [xTb.c(kt)])
        P.fence([xin], [xbcT, qkT, hidT])
        for li in range(nlayers):
            layer(li)
        P.dma("sp", oT_d[u], xT[:].rearrange("p k t -> p (k t)"), reads=[xT], sem_buf=xT)

    P.emit()
    es.close()
    return nc, list(dbg_out.keys())


def _x_units(x, core):
    units = []
    for s in range(2):
        b = 2 * core + s
        for blk in range(NBLK):
            xb = x[b, blk * TB:(blk + 1) * TB, :]
            a = xb.T.reshape(KT, 128, TB).transpose(1, 0, 2)
            units.append(np.ascontiguousarray(a).reshape(128, KT * TB))
    return np.stack(units)


def _units_to_out(o_units, out, core):
    for s in range(2):
        b = 2 * core + s
        for blk in range(NBLK):
            a = o_units[s * NBLK + blk].reshape(128, KT, TB).transpose(1, 0, 2).reshape(D, TB)
            out[b, blk * TB:(blk + 1) * TB, :] = a.T


_CACHE = {}


def _get_program(key, *args):
    if key not in _CACHE:
        _CACHE[key] = build_program(*args)
    return _CACHE[key]


FUSED = True


def kernel(**inp):
    inp = {k: np.asarray(v, np.float32) for k, v in inp.items()}
    x = inp["x"]
    ncores = 8
    cst = _make_cst()
    lay = [_layer_arrays(inp, l) for l in range(DEPTH)]
    n_units = 2 * NBLK
    seq_starts = {0, NBLK}
    out = np.zeros((BATCH, SEQ, D), np.float32)
    if FUSED:
        nc, _ = _get_program("fused", n_units, seq_starts, DEPTH, True)
        in_maps = []
        for c in range(ncores):
            m = {"xT": _x_units(x, c), "cst": cst}
            for l in range(DEPTH):
                for n, _sh in WSHAPES:
                    m[f"{n}{l}"] = lay[l][n]
                m[f"prm{l}"] = lay[l]["prm"]
                m[f"dexp{l}"] = lay[l]["dexp"]
            in_maps.append(m)
        res = run_bass_kernel_spmd(nc, in_maps, core_ids=list(range(ncores)))
        for c in range(ncores):
            _units_to_out(res.results[c]["oT"], out, c)
        return out
    cur = [_x_units(x, c) for c in range(ncores)]
    for l in range(DEPTH):
        nc, _ = _get_program(("layer", l == 0), n_units, seq_starts, 1, l == 0)
        in_maps = []
        for c in range(ncores):
            m = {"xT": cur[c], "cst": cst}
            for n, _sh in WSHAPES:
                m[f"{n}0"] = lay[l][n]
            m["prm0"] = lay[l]["prm"]
            m["dexp0"] = lay[l]["dexp"]
            in_maps.append(m)
        res = run_bass_kernel_spmd(nc, in_maps, core_ids=list(range(ncores)))
        cur = [np.asarray(res.results[c]["oT"], np.float32) for c in range(ncores)]
    for c in range(ncores):
        _units_to_out(cur[c], out, c)
    return out
```

```python
import numpy as np
from contextlib import ExitStack
import concourse.bass as bass
import concourse.mybir as mybir
from concourse.bass_utils import run_bass_kernel_spmd
from concourse.alu_op_type import AluOpType as ALU

AF = mybir.ActivationFunctionType
AX = mybir.AxisListType
F32 = mybir.dt.float32
BF16 = mybir.dt.bfloat16

DEPTH = 2
D = 1024
KT = 8
SEQ = 2048
BATCH = 16
TB = 512
NTT = 4
NBLK = SEQ // TB
M_H, DQK, DV = 8, 64, 128
S_H, S_P, S_G, S_N = 16, 64, 4, 128
DFF = 2752
NJ = 22
D_IN = 8224
ALPHA = (2 * DEPTH) ** 0.25
LN_EPS = 1e-5
RMS_EPS = 1e-6
NEG_BIG = -1e30

ENGS = ("pe", "act", "dve", "pool", "sp")
RELAX = ()
SCHEDULE = True


class DG:
    def __init__(self, name):
        self.name = name
        self.sem = None
        self.count = 0


class Buf:
    def __init__(self, h, name, ncell=1):
        self.h = h
        self.name = name
        self.ncell = ncell
        self.lw = [None] * ncell
        self.rd = [[] for _ in range(ncell)]
        self.dg = None

    def __getitem__(self, k):
        return self.h[k]

    def c(self, *idx):
        return (self, list(idx))


def _cells(x):
    if isinstance(x, Buf):
        return [(x, i) for i in range(x.ncell)]
    b, idx = x
    return [(b, i) for i in idx]


class Op:
    __slots__ = ("eng", "fn", "idx", "deps", "sig", "signo", "dma", "dg", "dval", "cost", "alld")


class Prog:
    def __init__(self, nc, es):
        self.nc = nc
        self.es = es
        self.ops = []
        self.dgs = []
        self.nsig = {e: 0 for e in ENGS}

    def sbuf(self, name, shape, dtype, ncell=1):
        h = self.es.enter_context(self.nc.sbuf_tensor("t_" + name, list(shape), dtype))
        return Buf(h, name, ncell)

    def psum(self, name, shape, dtype, ncell=1):
        h = self.es.enter_context(self.nc.psum_tensor("t_" + name, list(shape), dtype))
        return Buf(h, name, ncell)

    def op(self, eng, fn, reads=(), writes=(), dma=None, cost=0.5):
        o = Op()
        o.eng, o.fn, o.idx = eng, fn, len(self.ops)
        o.cost = cost
        o.sig, o.signo, o.dma, o.dg, o.dval = False, None, dma is not None, None, None
        deps = {}
        for r in reads:
            for (b, i) in _cells(r):
                if b.lw[i] is not None:
                    deps[b.lw[i]] = True
        for w in writes:
            for (b, i) in _cells(w):
                if b.lw[i] is not None:
                    deps.setdefault(b.lw[i], False)
                for j in b.rd[i]:
                    deps.setdefault(j, False)
        deps.pop(o.idx, None)
        o.deps = deps
        for r in reads:
            for (b, i) in _cells(r):
                b.rd[i].append(o.idx)
        for w in writes:
            for (b, i) in _cells(w):
                b.lw[i] = o.idx
                b.rd[i] = []
        if o.dma:
            if dma.dg is None:
                dma.dg = DG(dma.name)
                self.dgs.append(dma.dg)
            o.dg = dma.dg
            o.dg.count += 16
            o.dval = o.dg.count
        self.ops.append(o)
        return o

    def dma(self, eng, out_ap, in_ap, reads=(), writes=(), sem_buf=None):
        if sem_buf is None:
            sem_buf = _cells(writes[0])[0][0] if writes else _cells(reads[0])[0][0]
        nbytes = 1
        for d_ in in_ap.shape:
            nbytes *= d_
        nbytes *= 4 if in_ap.dtype == F32 else 2
        return self.op(eng, lambda e: e.dma_start(out=out_ap, in_=in_ap),
                       reads=reads, writes=writes, dma=sem_buf, cost=1.5 + nbytes / 200e3)

    def fence(self, old, new):
        s = set()
        for b in old:
            for i in range(b.ncell):
                if b.lw[i] is not None:
                    s.add(b.lw[i])
                s.update(b.rd[i])
        for b in new:
            for i in range(b.ncell):
                b.rd[i].extend(s)

    def schedule(self):
        import heapq
        ops = self.ops
        n = len(ops)
        succ = [[] for _ in range(n)]
        indeg = [0] * n
        for o in ops:
            for j in o.deps:
                succ[j].append(o.idx)
                indeg[o.idx] += 1
        ready_t = [0.0] * n
        fin = [0.0] * n
        heaps = {e: [] for e in ENGS}
        free = {e: 0.0 for e in ENGS}
        for o in ops:
            if indeg[o.idx] == 0:
                heapq.heappush(heaps[o.eng], o.idx)
        out = []
        LAT = 0.2
        WIN = 48
        while len(out) < n:
            best = None
            for e in ENGS:
                h = heaps[e]
                if not h:
                    continue
                cands = heapq.nsmallest(WIN, h)
                bi, bs = None, None
                for i in cands:
                    st = max(ready_t[i], free[e])
                    if bs is None or st < bs - 1e-9:
                        bi, bs = i, st
                if best is None or bs < best[0] - 1e-9 or (abs(bs - best[0]) <= 1e-9 and bi < best[1]):
                    best = (bs, bi, e)
            bs, bi, e = best
            heaps[e].remove(bi)
            heapq.heapify(heaps[e])
            o = ops[bi]
            f = bs + o.cost
            free[e] = bs + (0.15 if o.dma else o.cost)
            fin[bi] = f
            out.append(o)
            for k in succ[bi]:
                ready_t[k] = max(ready_t[k], f + LAT)
                indeg[k] -= 1
                if indeg[k] == 0:
                    heapq.heappush(heaps[ops[k].eng], k)
        self.sim_time = max(fin) if fin else 0.0
        return out

    def emit(self):
        nc, es, ops = self.nc, self.es, self.ops
        sched = self.schedule() if SCHEDULE else None
        for o in ops:
            need = []
            for j, raw in o.deps.items():
                p = ops[j]
                if p.dma:
                    need.append(j)
                    continue
                if p.eng == o.eng and not o.dma:
                    if o.eng == "pe" or (not raw and o.eng in RELAX):
                        continue
                need.append(j)
            o.deps = need
            for j in need:
                if not ops[j].dma:
                    ops[j].sig = True
        for o in (sched if sched is not None else ops):
            if o.sig and not o.dma:
                self.nsig[o.eng] += 1
                o.signo = self.nsig[o.eng]
        esem = {e: es.enter_context(nc.semaphore("s_" + e)) for e in ENGS}
        for g in self.dgs:
            g.sem = es.enter_context(nc.semaphore("d_" + g.name))
        streams = {e: [] for e in ENGS}
        for o in (sched if sched is not None else ops):
            streams[o.eng].append(o)
        block = es.enter_context(nc.Block())
        prog = self

        def run(eng_name, engine):
            seen = {e: 0 for e in ENGS}
            dseen = {}
            for o in streams[eng_name]:
                wl, dl = {}, {}
                for j in o.deps:
                    p = ops[j]
                    if p.dma:
                        k = id(p.dg)
                        if dseen.get(k, 0) < p.dval and (k not in dl or dl[k][1] < p.dval):
                            dl[k] = (p.dg, p.dval)
                    elif seen[p.eng] < p.signo:
                        wl[p.eng] = max(wl.get(p.eng, 0), p.signo)
                for e, v in wl.items():
                    engine.wait_ge(esem[e], v)
                    seen[e] = v
                for k, (g, v) in dl.items():
                    engine.wait_ge(g.sem, v)
                    dseen[k] = v
                ins = o.fn(engine)
                if o.dma:
                    ins.then_inc(o.dg.sem, 16)
                elif o.sig:
                    ins.then_inc(esem[eng_name], 1)
            if eng_name == "sp":
                for e in ENGS:
                    if prog.nsig[e]:
                        engine.wait_ge(esem[e], prog.nsig[e])
                for g in prog.dgs:
                    engine.wait_ge(g.sem, g.count)

        @block.tensor
        def _(e):
            run("pe", e)

        @block.scalar
        def _(e):
            run("act", e)

        @block.vector
        def _(e):
            run("dve", e)

        @block.gpsimd
        def _(e):
            run("pool", e)

        @block.sync
        def _(e):
            run("sp", e)


C_ID, C_M01, C_NEG, C_ONE, C_BLK, C_RST, C_SEL, C_O16 = 0, 128, 256, 384, 512, 528, 1040, 1552
NC_CST = 1552 + 512

_off = {}
_o = 0
for _n, _w in (("mcw", 32), ("mcb", 8), ("scw", 64), ("scb", 16), ("fcw", 132), ("fcb", 44),
               ("ln1g", 8), ("ln1b", 8), ("ln2g", 8), ("ln2b", 8), ("mng", 8), ("sng", 8),
               ("ib", 1), ("fb", 1), ("dtb", 1), ("alog", 1), ("inlg", 8), ("inlb", 8)):
    _off[_n] = _o
    _o += _w
NPRM = _o

Q0, K0, V0, O0, I0, F0, Z0, X0, DT0, GA0, GB0 = 0, 512, 1024, 2048, 3072, 3080, 3088, 4112, 6160, 6176, 7200


def _slab(wcols):
    n = wcols.shape[1] // 512
    a = wcols.reshape(KT, 128, n, 512).transpose(2, 1, 0, 3)
    return np.ascontiguousarray(a).reshape(n, 128, KT * 512)


def _pt(v, ntile):
    return np.ascontiguousarray(v.reshape(ntile, 128).T)


def _make_cst():
    c = np.zeros((128, NC_CST), np.float32)
    c[:, C_ID:C_ID + 128] = np.eye(128, dtype=np.float32)
    s = np.arange(128)[:, None]
    t = np.arange(128)[None, :]
    c[:, C_M01:C_M01 + 128] = (s <= t).astype(np.float32)
    c[:, C_NEG:C_NEG + 128] = np.where(s <= t, 0.0, -30000.0).astype(np.float32)
    c[:, C_ONE:C_ONE + 128] = 1.0
    c[0:16, C_BLK:C_BLK + 16] = np.eye(16, dtype=np.float32)
    r = np.ones((16, 512), np.float32)
    r[:, 0::128] = 0.0
    c[0:16, C_RST:C_RST + 512] = r
    sel = np.zeros((8, 4, 128), np.float32)
    for hp in range(4):
        sel[2 * hp, hp, 0:64] = 1.0
        sel[2 * hp + 1, hp, 64:128] = 1.0
    c[0:8, C_SEL:C_SEL + 512] = sel.reshape(8, 512)
    c[0:16, C_O16:C_O16 + 512] = 1.0
    return c


def _layer_arrays(inp, l):
    f32 = np.float32
    w_in = inp["w_in"][l]
    out = {}
    cols = [w_in[:, Q0:Q0 + 1024], w_in[:, X0:X0 + 2048], w_in[:, GA0:GA0 + 1024], w_in[:, GB0:GB0 + 1024],
            w_in[:, V0:V0 + 1024], w_in[:, O0:O0 + 1024], w_in[:, Z0:Z0 + 1024]]
    out["win"] = _slab(np.concatenate(cols, axis=1))
    mini = np.concatenate([w_in[:, I0:I0 + 8], w_in[:, F0:F0 + 8], w_in[:, DT0:DT0 + 16]], axis=1)
    out["winm"] = np.ascontiguousarray(mini.reshape(KT, 128, 32).transpose(1, 0, 2)).reshape(128, 256)
    out["pa"] = _slab(inp["p_a"][l])
    out["pb"] = _slab(inp["p_b"][l])
    out["wo"] = _slab(inp["w_out"][l])
    w_up = inp["w_up"][l]
    perm = []
    for j in range(NJ):
        m = 128 if j < 21 else 64
        perm += list(range(128 * j, 128 * j + m)) + list(range(DFF + 128 * j, DFF + 128 * j + m))
    wup_p = np.zeros((1024, 11 * 512), f32)
    wup_p[:, :len(perm)] = w_up[:, perm]
    out["wup"] = _slab(wup_p)
    wd = np.zeros((NJ * 128, 1024), f32)
    wd[:DFF] = inp["w_down"][l]
    out["wdn"] = np.ascontiguousarray(wd.reshape(NJ, 128, 8, 128).transpose(2, 1, 0, 3)).reshape(8, 128, NJ * 128)
    prm = np.zeros((128, NPRM), f32)

    def put(name, arr):
        a = np.asarray(arr, f32)
        prm[:a.shape[0], _off[name]:_off[name] + a.shape[1]] = a

    mcw = inp["m_conv_w"][l]
    put("mcw", mcw.T.reshape(8, 128, 4).transpose(1, 0, 2).reshape(128, 32))
    put("mcb", _pt(inp["m_conv_b"][l], 8))
    scw = inp["s_conv_w"][l]
    put("scw", scw.T.reshape(16, 128, 4).transpose(1, 0, 2).reshape(128, 64))
    put("scb", _pt(inp["s_conv_b"][l], 16))
    fcw = inp["f_conv_w"][l]
    fcb = inp["f_conv_b"][l]
    fw = np.zeros((128, NJ, 2, 3), f32)
    fb = np.zeros((128, NJ, 2), f32)
    for j in range(NJ):
        m = 128 if j < 21 else 64
        fw[:m, j, 0, :] = fcw[:, 128 * j:128 * j + m].T
        fw[:m, j, 1, :] = fcw[:, DFF + 128 * j:DFF + 128 * j + m].T
        fb[:m, j, 0] = fcb[128 * j:128 * j + m]
        fb[:m, j, 1] = fcb[DFF + 128 * j:DFF + 128 * j + m]
    put("fcw", fw.reshape(128, 132))
    put("fcb", fb.reshape(128, 44))
    put("ln1g", _pt(inp["ln1_g"][l], 8))
    put("ln1b", _pt(inp["ln1_b"][l], 8))
    put("ln2g", _pt(inp["ln2_g"][l], 8))
    put("ln2b", _pt(inp["ln2_b"][l], 8))
    put("mng", _pt(inp["m_norm_g"][l], 8))
    put("sng", _pt(inp["s_norm_g"][l], 8))
    put("ib", inp["m_i_bias"][l].reshape(8, 1))
    put("fb", inp["m_f_bias"][l].reshape(8, 1))
    put("dtb", inp["s_dt_bias"][l].reshape(16, 1))
    put("alog", inp["s_a_log"][l].reshape(16, 1))
    put("inlg", _pt(inp["in_ln_g"], 8))
    put("inlb", _pt(inp["in_ln_b"], 8))
    out["prm"] = prm
    out["dexp"] = np.ascontiguousarray(np.repeat(inp["s_d"][l], S_P).reshape(1, 1024).astype(f32))
    return out


WSHAPES = (("win", [16, 128, 4096]), ("winm", [128, 256]), ("pa", [2, 128, 4096]), ("pb", [2, 128, 4096]),
           ("wo", [2, 128, 4096]), ("wup", [11, 128, 4096]), ("wdn", [8, 128, NJ * 128]))


def build_program(n_units, seq_starts, nlayers, entry_ln, dbg=None, stop=None):
    nc = bass.Bass("TRN2", target_bir_lowering=False)
    es = ExitStack()
    P = Prog(nc, es)
    dbg = dbg or {}
    dbg_out = {}

    def din(name, shape):
        return nc.dram_tensor(name, list(shape), F32, kind="ExternalInput").ap()

    xT_d = din("xT", [n_units, 128, KT * TB])
    oT_d = nc.dram_tensor("oT", [n_units, 128, KT * TB], F32, kind="ExternalOutput").ap()
    cst_d = din("cst", [128, NC_CST])
    Wd, Sd = [], []
    for li in range(nlayers):
        w = {n: din(f"{n}{li}", sh) for n, sh in WSHAPES}
        w["prm"] = din(f"prm{li}", [128, NPRM])
        w["dexp"] = din(f"dexp{li}", [1, 1024])
        Wd.append(w)
        s = {}
        for n, sh in WSHAPES:
            ncell = sh[0] if len(sh) == 3 else 1
            s[n] = Buf(nc.dram_tensor(f"s_{n}{li}", list(sh), BF16).ap(), f"s_{n}{li}", ncell)
        Sd.append(s)

    xT = P.sbuf("xT", [128, KT, TB], F32, KT)
    xTb = P.sbuf("xTb", [128, KT, TB], BF16, KT)
    NWS = 3
    WS = [P.sbuf(f"ws{i}", [128, KT, 512], BF16) for i in range(NWS)]
    ar1 = es.enter_context(nc.sbuf_tensor("ar1", [128, 12288], BF16))
    ar2 = es.enter_context(nc.sbuf_tensor("ar2", [128, 11264], BF16))
    xbcT = Buf(ar1[:, 0:8192].rearrange("p (k t) -> p k t", k=16), "xbcT", 16)
    qkT = Buf(ar1[:, 8192:12288].rearrange("p (k t) -> p k t", k=8), "qkT", 8)
    hidT = Buf(ar1[:, 0:11264].rearrange("p (k t) -> p k t", k=NJ), "hidT", NJ)
    stg = [Buf(ar1[:, 0:8192].bitcast(F32), "stg0"), Buf(ar2[:, 0:8192].bitcast(F32), "stg1")]
    xin = Buf(ar1[:, 0:8192].bitcast(F32).rearrange("p (k t) -> p k t", k=KT), "xin", KT)
    WDb = [Buf(ar2[:, i * 2816:(i + 1) * 2816].rearrange("p (j c) -> p j c", j=NJ), f"wd{i}") for i in range(4)]
    v_aug = Buf(ar2[:, 0:4160].rearrange("p (t h c) -> p t h c", t=NTT, h=M_H), "v_aug", NTT)
    sig_o = Buf(ar2[:, 4160:8256].rearrange("p (t c) -> p t c", t=NTT), "sig_o", NTT)
    silu_z = P.sbuf("silu_z", [128, NTT, 1024], BF16, NTT)
    h_aT = P.sbuf("h_aT", [128, KT, TB], BF16, KT)
    h_bT = P.sbuf("h_bT", [128, KT, TB], BF16, KT)
    mergedT = Buf(silu_z.h[:].rearrange("p t c -> p (t c)").rearrange("p (k t) -> p k t", k=KT), "mergedT", KT)
    T4 = [P.sbuf(f"t4_{i}", [128, 1040], F32) for i in range(4)]
    cst = P.sbuf("cst", [128, NC_CST], F32)
    ident_b = P.sbuf("ident_b", [128, 128], BF16)
    ones_b = P.sbuf("ones_b", [128, 128], BF16)
    negm4 = P.sbuf("negm4", [128, 512], BF16)
    mask_m = P.sbuf("mask_m", [128, 128], F32)
    prm = [P.sbuf(f"prm{li}", [128, NPRM], F32) for li in range(nlayers)]
    nfb = [P.sbuf(f"nfb{li}", [8, 1], F32) for li in range(nlayers)]
    negA = [P.sbuf(f"negA{li}", [16, 1], F32) for li in range(nlayers)]
    halo_qk = [P.sbuf(f"hqk{li}", [128, 8, 3], F32, 8) for li in range(nlayers)]
    halo_x = [P.sbuf(f"hx{li}", [128, 16, 3], F32, 16) for li in range(nlayers)]
    halo_f = [P.sbuf(f"hf{li}", [128, 2 * NJ, 2], F32, 2 * NJ) for li in range(nlayers)]
    Bc = [P.sbuf(f"bc{li}", [8, 1], F32) for li in range(nlayers)]
    Rc = [P.sbuf(f"rc{li}", [8, 1], F32) for li in range(nlayers)]
    Sst = [P.sbuf(f"sst{li}", [128, 4, 130], F32) for li in range(nlayers)]
    Hst = [P.sbuf(f"hst{li}", [128, 16, 64], F32) for li in range(nlayers)]
    Hb = [P.sbuf(f"hb{li}", [128, 16, 64], BF16) for li in range(nlayers)]
    Sb = P.sbuf("sb", [128, 4, 130], BF16)
    G = [P.sbuf(f"g{i}", [16, 512], F32) for i in range(4)]
    Gdt = P.sbuf("gdt", [16, 512], F32)
    sm_cm = P.sbuf("sm_cm", [16, 4], F32)
    sm_R = P.sbuf("sm_R", [16, 4], F32)
    sm_Rp = P.sbuf("sm_Rp", [16, 4], F32)
    sm_gm = P.sbuf("sm_gm", [16, 4], F32)
    sm_cd = P.sbuf("sm_cd", [16, 4], F32)
    sm_dg = P.sbuf("sm_dg", [16, 4, 16], F32)
    gtok = P.sbuf("gtok", [128, NTT, 16], F32)
    gam_bc = P.sbuf("gam_bc", [128, 4, 4], F32)
    stok = P.sbuf("stok", [128, NTT, 64], F32)
    cd_bc = P.sbuf("cd_bc", [128, 4, 16], F32)
    k_tok = P.sbuf("k_tok", [128, 512], BF16)
    qz1 = P.sbuf("qz1", [128, 4, 512], BF16, 4)
    vw = P.sbuf("vw", [128, 8, 130], BF16)
    PT = [P.sbuf(f"pt{i}", [128, 2, 128], BF16) for i in range(2)]
    sm_d = P.sbuf("sm_d", [128, 8], F32)
    sm_ss = P.sbuf("sm_ss", [128, 8], F32)
    sm_ss2 = P.sbuf("sm_ss2", [128, 8], F32)
    junk = P.sbuf("junk", [128, 128], BF16)
    junk2 = P.sbuf("junk2", [128, 256], BF16)
    hgb = Buf(ar2[:, 8256:9280], "hgb")
    ynb = Buf(ar2[:, 9280:10304], "ynb")
    Dm = [P.sbuf(f"dm{li}", [128, 16, 64], BF16) for li in range(nlayers)]
    xs_tok = P.sbuf("xs_tok", [128, 1024], BF16)
    B_tok = P.sbuf("B_tok", [128, 512], BF16)
    xdt = P.sbuf("xdt", [128, 16, 64], BF16)
    xdte = P.sbuf("xdte", [128, 16, 64], BF16)
    CBm = P.sbuf("CBm", [128, 4, 128], F32)
    Zg = [P.sbuf(f"zg{i}", [128, 4, 128], F32) for i in range(2)]
    decb = [P.sbuf(f"dec{i}", [128, 4, 128], BF16) for i in range(2)]
    Gg = [P.sbuf(f"gg{i}", [128, 4, 128], BF16) for i in range(2)]
    PB = [P.psum(f"pb{i}", [128, 512], F32) for i in range(7)]
    PBT = P.psum("pbt", [128, 1024], BF16)

    ident_f = cst[:, C_ID:C_ID + 128]
    mask01 = cst[:, C_M01:C_M01 + 128]
    ones_f = cst[:, C_ONE:C_ONE + 128]

    def fsz(ap):
        n = 1
        for d_ in ap.shape[1:]:
            n *= d_
        return n

    def act(out, in_, func, reads, writes, bias=None, scale=None):
        kw = {}
        if bias is not None:
            kw["bias"] = bias
        if scale is not None:
            kw["scale"] = scale
        P.op("act", lambda e: e.activation(out=out, in_=in_, func=func, **kw), reads, writes,
             cost=0.22 + fsz(out) / 1400.0)

    def ecost(eng, out, mult=1.0):
        if eng == "pool":
            return 0.4 + mult * fsz(out) / 520.0
        if eng == "act":
            return 0.22 + mult * fsz(out) / 1400.0
        return 0.12 + mult * fsz(out) / 960.0

    def tt(eng, out, in0, in1, op, reads, writes):
        P.op(eng, lambda e: e.tensor_tensor(out=out, in0=in0, in1=in1, op=op), reads, writes, cost=ecost(eng, out))

    def ts(eng, out, in0, s1, op0, reads, writes, s2=None, op1=None):
        if op1 is None:
            P.op(eng, lambda e: e.tensor_scalar(out=out, in0=in0, scalar1=s1, scalar2=None, op0=op0), reads, writes,
                 cost=ecost(eng, out))
        else:
            P.op(eng, lambda e: e.tensor_scalar(out=out, in0=in0, scalar1=s1, scalar2=s2, op0=op0, op1=op1),
                 reads, writes, cost=ecost(eng, out))

    def stt(out, in0, scalar, in1, op0, op1, reads, writes):
        P.op("dve", lambda e: e.scalar_tensor_tensor(out=out, in0=in0, scalar=scalar, in1=in1, op0=op0, op1=op1),
             reads, writes, cost=ecost("dve", out))

    def cp(eng, out, in_, reads, writes):
        if eng == "act":
            P.op("act", lambda e: e.copy(out=out, in_=in_), reads, writes, cost=ecost("act", out))
        else:
            P.op(eng, lambda e: e.tensor_copy(out=out, in_=in_), reads, writes, cost=ecost(eng, out))

    def mm(out, lhsT, rhs, start, stop, reads, writes):
        nn = max(fsz(rhs), 64) / 2000.0
        if rhs.dtype == F32:
            nn *= 4
        P.op("pe", lambda e: e.matmul(out, lhsT=lhsT, rhs=rhs, start=start, stop=stop), reads, writes,
             cost=nn + 0.03)

    def tr(out, in_, ident, reads, writes):
        P.op("pe", lambda e: e.transpose(out, in_, ident), reads, writes, cost=0.09)

    def memset(eng, ap, val, writes):
        P.op(eng, lambda e: e.memset(ap, val), (), writes, cost=ecost(eng, ap))

    def recip(out, in_, reads, writes):
        P.op("dve", lambda e: e.reciprocal(out=out, in_=in_), reads, writes, cost=ecost("dve", out, 8.0))

    P.dma("sp", cst[:], cst_d, writes=[cst])
    for li in range(nlayers):
        P.dma("sp", prm[li][:], Wd[li]["prm"], writes=[prm[li]])
    cp("dve", ident_b[:], ident_f, [cst], [ident_b])
    cp("dve", ones_b[:], ones_f, [cst], [ones_b])
    for q in range(4):
        cp("dve", negm4[:, q * 128:(q + 1) * 128], cst[:, C_NEG:C_NEG + 128], [cst], [negm4])
    ts("dve", mask_m[:], mask01, DQK ** -0.5, ALU.mult, [cst], [mask_m])
    for li in range(nlayers):
        o = _off
        ts("dve", nfb[li][:], prm[li][0:8, o["fb"]:o["fb"] + 1], -1.0, ALU.mult, [prm[li]], [nfb[li]])
        act(negA[li][:], prm[li][0:16, o["alog"]:o["alog"] + 1], AF.Exp, [prm[li]], [negA[li]])
        ts("dve", negA[li][:], negA[li][:], -1.0, ALU.mult, [negA[li]], [negA[li]])
    for li in range(nlayers):
        dexp1 = T4[3]
        P.dma("sp", dexp1[:, 0:1024], Wd[li]["dexp"].partition_broadcast(128), writes=[dexp1])
        for h in range(16):
            e2 = h % 2
            ts("dve", Dm[li][:, h, :], cst[:, C_ID + 64 * e2:C_ID + 64 * e2 + 64], dexp1[:, h * 64:h * 64 + 1],
               ALU.mult, [cst, dexp1], [Dm[li]])
    memset("pool", vw[:], 0.0, [vw])
    for i in range(4):
        memset("pool", T4[i][:], 0.0, [T4[i]])
    for i in range(4):
        memset("pool", G[i][:], 0.0, [G[i]])
    for i in range(2):
        memset("pool", Zg[i][:], 0.0, [Zg[i]])
    memset("pool", qz1[:], 0.0, [qz1])

    jobs = []
    for li in range(nlayers):
        for n, sh in WSHAPES:
            if n == "winm":
                jobs.append((Wd[li][n], Sd[li][n].h, [Sd[li][n]], 256, None, li))
            elif n == "wdn":
                for s in range(sh[0]):
                    jobs.append((Wd[li][n][s], Sd[li][n].h[s], [Sd[li][n].c(s)], 2816, None, li))
            else:
                for s in range(sh[0]):
                    sc = {"pa": "mng", "pb": "sng"}.get(n)
                    jobs.append((Wd[li][n][s], Sd[li][n].h[s], [Sd[li][n].c(s)], 4096, sc, li))
    cast_engs = ("act", "dve", "act")

    def pre_load(ji):
        src, dst, cells, F, sc, li = jobs[ji]
        st = stg[ji % 2]
        P.dma("sp", st[:, 0:F], src, writes=[st])

    def pre_cast_store(ji):
        src, dst, cells, F, sc, li = jobs[ji]
        st = stg[ji % 2]
        wb = WS[ji % NWS]
        wflat = wb[:].rearrange("p k c -> p (k c)")
        if sc is not None:
            g = prm[li][:, _off[sc]:_off[sc] + 8]
            tt("dve", wb[:], st[:, 0:4096].rearrange("p (k c) -> p k c", k=KT),
               g.unsqueeze(2).to_broadcast([128, KT, 512]), ALU.mult, [st, prm[li]], [wb])
        else:
            cp(cast_engs[ji % 3], wflat[:, 0:F], st[:, 0:F], [st], [wb])
        P.dma("sp", dst, wflat[:, 0:F], reads=[wb], writes=cells, sem_buf=wb)

    for ji in range(len(jobs) + 1):
        if ji < len(jobs):
            pre_load(ji)
        if ji >= 1:
            pre_cast_store(ji - 1)
    P.fence(stg, [xbcT, qkT, hidT, xin] + WDb + [v_aug, sig_o, hgb, ynb])

    ws_ctr = [0]

    def load_slab(li, name, s):
        wb = WS[ws_ctr[0] % NWS]
        ws_ctr[0] += 1
        sb = Sd[li][name]
        P.dma("sp", wb[:].rearrange("p k c -> p (k c)"), sb.h[s], reads=[sb.c(s)], writes=[wb])
        return wb

    gb_ctr = [0]

    def gbank():
        b = PB[gb_ctr[0] % 6]
        gb_ctr[0] += 1
        return b

    def layernorm(g_ap, b_ap, pbuf, src=None):
        src = xT if src is None else src
        ps_m, ps_q = PB[5], PB[6]
        for kt in range(KT):
            tb = T4[1 + 2 * (kt % 2)]
            tbv = tb[:].bitcast(BF16)
            act(tbv[:, 0:512], src[:, kt, :], AF.Copy, [src.c(kt)], [tb])
            act(tbv[:, 512:1024], src[:, kt, :], AF.Square, [src.c(kt)], [tb])
            mm(ps_m[:, :], ones_b[:], tbv[:, 0:512], kt == 0, kt == KT - 1, [ones_b, tb], [ps_m])
            mm(ps_q[:, :], ones_b[:], tbv[:, 512:1024], kt == 0, kt == KT - 1, [ones_b, tb], [ps_q])
        mean, var = T4[0], T4[2]
        act(mean[:, 0:512], ps_m[:, :], AF.Copy, [ps_m], [mean], scale=1.0 / D)
        tt("dve", var[:, 0:512], mean[:, 0:512], mean[:, 0:512], ALU.mult, [mean], [var])
        stt(var[:, 0:512], ps_q[:, :], 1.0 / D, var[:, 0:512], ALU.mult, ALU.subtract, [ps_q, var], [var])
        ts("dve", var[:, 0:512], var[:, 0:512], LN_EPS, ALU.add, [var], [var])
        act(var[:, 0:512], var[:, 0:512], AF.Ln, [var], [var])
        act(var[:, 0:512], var[:, 0:512], AF.Exp, [var], [var], scale=-0.5)
        for kt in range(KT):
            tmp = T4[1 + 2 * (kt % 2)]
            tt("pool", tmp[:, 0:512], src[:, kt, :], mean[:, 0:512], ALU.subtract, [src.c(kt), mean], [tmp])
            tt("dve", tmp[:, 0:512], tmp[:, 0:512], var[:, 0:512], ALU.mult, [tmp, var], [tmp])
            act(xT[:, kt, :], tmp[:, 0:512], AF.Identity, [tmp, pbuf], [xT.c(kt)],
                bias=b_ap[:, kt:kt + 1], scale=g_ap[:, kt:kt + 1])
            cp("dve", xTb[:, kt, :], xT[:, kt, :], [xT.c(kt)], [xTb.c(kt)])

    acc_ctr = [0]

    def conv_pair(items, pbuf):
        accs = []
        for i, (ps, M, Kc, halo_buf, hcell, w_ap, b_ap) in enumerate(items):
            acc = T4[acc_ctr[0] % 4]
            acc_ctr[0] += 1
            Hh = Kc - 1
            act(acc[0:M, 0:512], ps[0:M, :], AF.Identity, [ps, pbuf], [acc],
                bias=b_ap[0:M, 0:1], scale=w_ap[0:M, Hh:Hh + 1])
            accs.append(acc)
        Kc = items[0][2]
        Hh = Kc - 1
        for k in range(Kc - 1):
            sh = Hh - k
            for i, (ps, M, _k, halo_buf, hcell, w_ap, b_ap) in enumerate(items):
                acc = accs[i]
                stt(acc[0:M, sh:512], ps[0:M, 0:512 - sh], w_ap[0:M, k:k + 1], acc[0:M, sh:512], ALU.mult, ALU.add,
                    [ps, pbuf, acc], [acc])
                stt(acc[0:M, 0:sh], halo_buf[0:M, hcell, k:Hh], w_ap[0:M, k:k + 1], acc[0:M, 0:sh], ALU.mult,
                    ALU.add, [halo_buf.c(hcell), pbuf, acc], [acc])
        for i, (ps, M, Kc_, halo_buf, hcell, w_ap, b_ap) in enumerate(items):
            cp("act", halo_buf[0:M, hcell, 0:Hh], ps[0:M, 512 - Hh:512], [ps], [halo_buf.c(hcell)])
        return accs

    def gemm_A(wb, c0, M, rhsbuf, ps):
        for kt in range(KT):
            mm(ps[0:M, :], wb[:, kt, c0:c0 + M], rhsbuf[:, kt, :], kt == 0, kt == KT - 1,
               [wb, rhsbuf.c(kt)], [ps])

    def mlstm_gates(li, ps_i, ps_f):
        o = _off
        pl = prm[li]
        act(G[0][0:8, :], ps_f[0:8, :], AF.Exp, [ps_f, nfb[li]], [G[0]], bias=nfb[li][:], scale=-1.0)
        act(G[0][0:8, :], G[0][0:8, :], AF.Ln, [G[0]], [G[0]], bias=1.0)
        P.op("dve", lambda e: e.tensor_tensor_scan(out=G[1][0:8, :], data0=cst[0:8, C_O16:C_O16 + 512],
                                                   data1=G[0][0:8, :], initial=Bc[li][:],
                                                   op0=ALU.mult, op1=ALU.subtract),
             [cst, G[0], Bc[li]], [G[1]])
        cp("pool", Bc[li][:], G[1][0:8, 511:512], [G[1]], [Bc[li]])
        yield
        stt(G[2][0:8, :], ps_i[0:8, :], pl[0:8, o["ib"]:o["ib"] + 1], G[1][0:8, :], ALU.add, ALU.subtract,
            [ps_i, pl, G[1]], [G[2]])
        P.op("dve", lambda e: e.tensor_reduce(out=sm_cm[0:8, :], in_=G[2][0:8, :].rearrange("p (c t) -> p c t", c=4),
                                              axis=AX.X, op=ALU.max), [G[2]], [sm_cm])
        P.op("dve", lambda e: e.tensor_tensor_scan(out=sm_R[0:8, :], data0=sm_cm[0:8, :], data1=sm_cm[0:8, :],
                                                   initial=Rc[li][:], op0=ALU.max, op1=ALU.max),
             [sm_cm, Rc[li]], [sm_R])
        cp("pool", sm_Rp[0:8, 0:1], Rc[li][:], [Rc[li]], [sm_Rp])
        cp("pool", sm_Rp[0:8, 1:4], sm_R[0:8, 0:3], [sm_R], [sm_Rp])
        cp("pool", Rc[li][:], sm_R[0:8, 3:4], [sm_R], [Rc[li]])
        yield
        tt("dve", sm_gm[0:8, :], sm_Rp[0:8, :], sm_R[0:8, :], ALU.subtract, [sm_Rp, sm_R], [sm_gm])
        act(sm_gm[0:8, :], sm_gm[0:8, :], AF.Exp, [sm_gm], [sm_gm])
        Rb = sm_R[0:8, :].unsqueeze(2).to_broadcast([8, 4, 128])
        g2v = G[2][0:8, :].rearrange("p (c t) -> p c t", c=4)
        g1v = G[1][0:8, :].rearrange("p (c t) -> p c t", c=4)
        g3v = G[3][0:8, :].rearrange("p (c t) -> p c t", c=4)
        tt("dve", g2v, g2v, Rb, ALU.subtract, [G[2], sm_R], [G[2]])
        act(G[2][0:8, :], G[2][0:8, :], AF.Exp, [G[2]], [G[2]])
        yield
        tt("dve", g3v, g1v, Rb, ALU.add, [G[1], sm_R], [G[3]])
        act(G[3][0:8, :], G[3][0:8, :], AF.Exp, [G[3]], [G[3]], scale=-1.0)
        yield
        ps_g = PB[2]
        for t4 in range(NTT):
            tsl = slice(t4 * 128, (t4 + 1) * 128)
            tr(ps_g[:, t4 * 16:t4 * 16 + 8], G[2][0:8, tsl], cst[0:8, C_ID:C_ID + 8], [G[2], cst], [ps_g])
            tr(ps_g[:, t4 * 16 + 8:t4 * 16 + 16], G[3][0:8, tsl], cst[0:8, C_ID:C_ID + 8], [G[3], cst], [ps_g])
        for hp in range(4):
            mm(ps_g[:, 64 + hp * 4:64 + hp * 4 + 4], cst[0:8, C_SEL + hp * 128:C_SEL + (hp + 1) * 128],
               sm_gm[0:8, :], True, True, [cst, sm_gm], [ps_g])
        cp("act", gtok[:].rearrange("p t c -> p (t c)"), ps_g[:, 0:64], [ps_g], [gtok])
        cp("act", gam_bc[:].rearrange("p h c -> p (h c)"), ps_g[:, 64:80], [ps_g], [gam_bc])
        yield

    import os as _os
    mstop = int(_os.environ.get("MSTOP", "0"))

    def mchk(k):
        if mstop == k:
            raise _Stop()

    def mlstm_chunk(li, c):
        tsl = slice(c * 128, (c + 1) * 128)
        S = Sst[li]
        for hp in range(4):
            tr(PBT[:, hp * 128:(hp + 1) * 128], qkT[:, 4 + hp, tsl], ident_b[:], [qkT.c(4 + hp), ident_b], [PBT])
        cp("act", k_tok[:], PBT[:, 0:512], [PBT], [k_tok])
        mchk(1)
        yield
        tt("pool", vw[:, :, 0:129], v_aug[:, c, :, 0:129],
           gtok[:, c, 0:8].unsqueeze(2).to_broadcast([128, 8, 129]), ALU.mult, [v_aug.c(c), gtok], [vw])
        tt("pool", S[:, :, 0:129], S[:, :, 0:129],
           gam_bc[:, :, c:c + 1].to_broadcast([128, 4, 129]), ALU.mult, [S, gam_bc], [S])
        act(Sb[:, :, 0:129], S[:, :, 0:129], AF.Copy, [S], [Sb], scale=DQK ** -0.5)
        osb = T4[0][:, 0:1040].rearrange("p (h c) -> p h c", h=8)
        mchk(2)
        yield
        for hp in range(4):
            X, Y, Ub = PB[0], PB[1], PB[2]
            pt = PT[hp % 2]
            QZ = (qkT, qz1)
            for e2 in range(2):
                mm(X[:, e2 * 128:(e2 + 1) * 128], qkT[:, 4 + hp, tsl], QZ[e2][:, hp, tsl], True, True,
                   [qkT.c(4 + hp), QZ[e2].c(hp)], [X])
            tt("dve", pt[:], X[:, 0:256].rearrange("p (h t) -> p h t", h=2),
               mask_m[:].unsqueeze(1).to_broadcast([128, 2, 128]), ALU.mult, [X, mask_m], [pt])
            mchk(3)
            yield
            for e2 in range(2):
                h = 2 * hp + e2
                mm(Y[:, e2 * 130:e2 * 130 + 129], pt[:, e2, :], vw[:, h, 0:129], True, False, [pt, vw], [Y])
                mm(Y[:, e2 * 130:e2 * 130 + 129], QZ[e2][:, hp, tsl], Sb[:, hp, 0:129], False, True,
                   [QZ[e2].c(hp), Sb], [Y])
            cp("act", T4[0][:, hp * 260:hp * 260 + 260], Y[:, 0:260], [Y], [T4[0]])
            mchk(4)
            yield
            mm(Ub[:, 0:260], k_tok[:, hp * 128:(hp + 1) * 128],
               vw[:, 2 * hp:2 * hp + 2, :].rearrange("p h c -> p (h c)"), True, True, [k_tok, vw], [Ub])
            tt("dve", S[0:64, hp, 0:129], S[0:64, hp, 0:129], Ub[0:64, 0:129], ALU.add, [S, Ub], [S])
            tt("dve", S[64:128, hp, 0:129], S[64:128, hp, 0:129], Ub[64:128, 130:259], ALU.add, [S, Ub], [S])
            mchk(5)
            yield
        mchk(6)

    def mlstm_tail(li, c):
        tsl = slice(c * 128, (c + 1) * 128)
        osb = T4[0][:, 0:1040].rearrange("p (h c) -> p h c", h=8)
        act(sm_d[:], osb[:, :, 128], AF.Abs, [T4[0]], [sm_d])
        tt("dve", sm_d[:], sm_d[:], gtok[:, c, 8:16], ALU.max, [sm_d, gtok], [sm_d])
        recip(sm_d[:], sm_d[:], [sm_d], [sm_d])
        hv = T4[1][:, 0:1024].rearrange("p (h c) -> p h c", h=8)
        tt("dve", hv, osb[:, :, 0:128], sm_d[:].unsqueeze(2).to_broadcast([128, 8, 128]), ALU.mult,
           [T4[0], sm_d], [T4[1]])
        yield
        for h in range(8):
            P.op("act", lambda e, h=h: e.activation(out=junk[:, 0:128], in_=T4[1][:, h * 128:(h + 1) * 128],
                                                    func=AF.Square, accum_out=sm_ss[:, h:h + 1]),
                 [T4[1]], [junk, sm_ss])
            if h % 4 == 3:
                yield
        ts("dve", sm_ss[:], sm_ss[:], 1.0 / DV, ALU.mult, [sm_ss], [sm_ss], s2=RMS_EPS, op1=ALU.add)
        act(sm_ss[:], sm_ss[:], AF.Ln, [sm_ss], [sm_ss])
        act(sm_ss[:], sm_ss[:], AF.Exp, [sm_ss], [sm_ss], scale=-0.5)
        yield
        tt("dve", hv, hv, sm_ss[:].unsqueeze(2).to_broadcast([128, 8, 128]), ALU.mult, [T4[1], sm_ss], [T4[1]])
        tt("pool", hgb[:, :], T4[1][:, 0:1024], sig_o[:, c, :], ALU.mult, [T4[1], sig_o.c(c)], [hgb])
        yield
        for h in range(8):
            tr(PBT[:, h * 128:(h + 1) * 128], hgb[:, h * 128:(h + 1) * 128], ident_b[:], [hgb, ident_b], [PBT])
        cp("act", h_aT[:, :, tsl], PBT[:, :].rearrange("p (h t) -> p h t", h=8), [PBT], [h_aT])
        yield

    def ssd_gates(li):
        Gq = (Gdt, G[1], G[2], G[3])
        ts("dve", G[1][:], Gdt[:], negA[li][:], ALU.mult, [Gdt, negA[li]], [G[1]])
        P.op("dve", lambda e: e.tensor_tensor_scan(out=G[2][:], data0=cst[0:16, C_RST:C_RST + 512], data1=G[1][:],
                                                   initial=0.0, op0=ALU.mult, op1=ALU.add),
             [cst, G[1]], [G[2]])
        act(G[3][:], G[2][:], AF.Exp, [G[2]], [G[3]])
        yield
        g2v = G[2][:].rearrange("p (c t) -> p c t", c=4)
        g1v = G[1][:].rearrange("p (c t) -> p c t", c=4)
        tt("dve", g1v, g2v[:, :, 127:128].to_broadcast([16, 4, 128]), g2v, ALU.subtract, [G[2]], [G[1]])
        act(G[1][:], G[1][:], AF.Exp, [G[1]], [G[1]])
        yield
        act(sm_cd[:], g2v[:, :, 127], AF.Exp, [G[2]], [sm_cd])
        tt("dve", sm_dg[:], cst[0:16, C_BLK:C_BLK + 16].unsqueeze(1).to_broadcast([16, 4, 16]),
           sm_cd[:].unsqueeze(2).to_broadcast([16, 4, 16]), ALU.mult, [cst, sm_cd], [sm_dg])
        ps_g = PB[6]
        for t4 in range(NTT):
            tsl = slice(t4 * 128, (t4 + 1) * 128)
            for q, gi in enumerate((0, 3, 1, 2)):
                tr(ps_g[:, t4 * 64 + q * 16:t4 * 64 + q * 16 + 16], Gq[gi][:, tsl], cst[0:16, C_ID:C_ID + 16],
                   [Gq[gi], cst], [ps_g])
        mm(ps_g[:, 256:320], cst[0:16, C_ONE:C_ONE + 128], sm_dg[:].rearrange("p c h -> p (c h)"), True, True,
           [cst, sm_dg], [ps_g])
        cp("act", stok[:].rearrange("p t c -> p (t c)"), ps_g[:, 0:256], [ps_g], [stok])
        ts("dve", stok[:, :, 48:64], stok[:, :, 48:64], -1.0, ALU.mult, [stok], [stok])
        cp("act", cd_bc[:].rearrange("p c h -> p (c h)"), ps_g[:, 256:320], [ps_g], [cd_bc])
        yield

    def ssd_chunk(li, c):
        tsl = slice(c * 128, (c + 1) * 128)
        H = Hst[li]
        for kt in range(8):
            tr(PBT[:, kt * 128:(kt + 1) * 128], xbcT[:, kt, tsl], ident_b[:], [xbcT.c(kt), ident_b], [PBT])
        cp("act", xs_tok[:], PBT[:, :], [PBT], [xs_tok])
        yield
        for g in range(4):
            tr(PBT[:, g * 128:(g + 1) * 128], xbcT[:, 8 + g, tsl], ident_b[:], [xbcT.c(8 + g), ident_b], [PBT])
        cp("act", B_tok[:], PBT[:, 0:512], [PBT], [B_tok])
        yield
        xsv = xs_tok[:].rearrange("p (h c) -> p h c", h=16)
        tt("pool", xdt[:], xsv, stok[:, c, 0:16].unsqueeze(2).to_broadcast([128, 16, 64]), ALU.mult,
           [xs_tok, stok], [xdt])
        tt("pool", xdte[:], xdt[:], stok[:, c, 32:48].unsqueeze(2).to_broadcast([128, 16, 64]), ALU.mult,
           [xdt, stok], [xdte])
        yield
        for g in range(4):
            mm(PB[3][:, g * 128:(g + 1) * 128], xbcT[:, 8 + g, tsl], xbcT[:, 12 + g, tsl], True, True,
               [xbcT.c(8 + g), xbcT.c(12 + g)], [PB[3]])
        tt("dve", CBm[:], PB[3][:, :].rearrange("p (g t) -> p g t", g=4),
           mask01.unsqueeze(1).to_broadcast([128, 4, 128]), ALU.mult, [PB[3], cst], [CBm])
        yield
        for g in range(4):
            zg, A_, dec, gg, Yb = Zg[g % 2], PB[4], decb[g % 2], Gg[g % 2], PB[5]
            tt("pool", zg[0:16], G[2][:, tsl].unsqueeze(1).to_broadcast([16, 4, 128]),
               cst[0:16, C_BLK + 4 * g:C_BLK + 4 * g + 4].unsqueeze(2).to_broadcast([16, 4, 128]), ALU.mult,
               [G[2], cst], [zg])
            mm(A_[:, :], ones_f, zg[:].rearrange("p h t -> p (h t)"), True, False,
               [cst, zg], [A_])
            mm(A_[:, :], ident_b[:], negm4[:], False, True, [ident_b, negm4], [A_])
            yield
            for hh in range(4):
                h = 4 * g + hh
                act(dec[:, hh, :], A_[:, hh * 128:(hh + 1) * 128], AF.Exp, [A_, stok], [dec],
                    bias=stok[:, c, 48 + h:49 + h])
            tt("dve", gg[:], dec[:], CBm[:, g, :].unsqueeze(1).to_broadcast([128, 4, 128]), ALU.mult,
               [dec, CBm], [gg])
            yield
            for hh in range(4):
                h = 4 * g + hh
                mm(Yb[:, hh * 64:(hh + 1) * 64], gg[:, hh, :], xdt[:, h, :], True, False, [gg, xdt], [Yb])
                mm(Yb[:, hh * 64:(hh + 1) * 64], xbcT[:, h // 2, tsl], Dm[li][:, h, :], False, True,
                   [xbcT.c(h // 2), Dm[li]], [Yb])
            mm(Yb[:, 256:512], xbcT[:, 12 + g, tsl], Hb[li][:, 4 * g:4 * g + 4, :].rearrange("p h c -> p (h c)"),
               True, True, [xbcT.c(12 + g), Hb[li]], [Yb])
            yv = T4[2][:, g * 256:(g + 1) * 256]
            cp("act", yv, Yb[:, 256:512], [Yb], [T4[2]])
            yield
            yv3 = yv.rearrange("p (h c) -> p h c", h=4)
            tt("dve", yv3, yv3, stok[:, c, 16 + 4 * g:16 + 4 * g + 4].unsqueeze(2).to_broadcast([128, 4, 64]),
               ALU.mult, [T4[2], stok], [T4[2]])
            tt("dve", yv, yv, Yb[:, 0:256], ALU.add, [T4[2], Yb], [T4[2]])
            yield
            U = PB[6]
            mm(U[:, 0:256], B_tok[:, g * 128:(g + 1) * 128],
               xdte[:, 4 * g:4 * g + 4, :].rearrange("p h c -> p (h c)"), True, True, [B_tok, xdte], [U])
            hv = H[:, 4 * g:4 * g + 4, :]
            tt("pool", hv, hv, cd_bc[:, c, 4 * g:4 * g + 4].unsqueeze(2).to_broadcast([128, 4, 64]), ALU.mult,
               [H, cd_bc], [H])
            tt("dve", hv, hv, U[:, 0:256].rearrange("p (h c) -> p h c", h=4), ALU.add, [H, U], [H])
            yield
        cp("act", Hb[li][:], H[:], [H], [Hb[li]])
        yield

    def ssd_tail(li, c):
        tsl = slice(c * 128, (c + 1) * 128)
        tt("pool", T4[3][:, 0:1024], T4[2][:, 0:1024], silu_z[:, c, :], ALU.mult, [T4[2], silu_z.c(c)], [T4[3]])
        yield
        for g in range(4):
            P.op("act", lambda e, g=g: e.activation(out=junk2[:, 0:256], in_=T4[3][:, g * 256:(g + 1) * 256],
                                                    func=AF.Square, accum_out=sm_ss2[:, g:g + 1]),
                 [T4[3]], [junk2, sm_ss2])
        yield
        ts("dve", sm_ss2[:, 0:4], sm_ss2[:, 0:4], 1.0 / 256, ALU.mult, [sm_ss2], [sm_ss2], s2=RMS_EPS, op1=ALU.add)
        act(sm_ss2[:, 0:4], sm_ss2[:, 0:4], AF.Ln, [sm_ss2], [sm_ss2])
        act(sm_ss2[:, 0:4], sm_ss2[:, 0:4], AF.Exp, [sm_ss2], [sm_ss2], scale=-0.5)
        yield
        tt("dve", ynb[:, :].rearrange("p (g c) -> p g c", g=4),
           T4[3][:, 0:1024].rearrange("p (g c) -> p g c", g=4),
           sm_ss2[:, 0:4].unsqueeze(2).to_broadcast([128, 4, 256]), ALU.mult, [T4[3], sm_ss2], [ynb])
        yield
        for kt in range(8):
            tr(PBT[:, kt * 128:(kt + 1) * 128], ynb[:, kt * 128:(kt + 1) * 128], ident_b[:], [ynb, ident_b], [PBT])
        cp("act", h_bT[:, :, tsl], PBT[:, :].rearrange("p (h t) -> p h t", h=8), [PBT], [h_bT])
        yield

    dexp_loaded = [False]

    class _Stop(Exception):
        pass

    def chk(name):
        if stop == name:
            raise _Stop()

    def layer(li):
        try:
            layer_(li)
        except _Stop:
            pass

    def layer_(li):
        o = _off
        pl = prm[li]
        chk("start")
        P.fence([hidT], [xbcT, qkT])
        P.fence(WDb, [v_aug, sig_o, hgb, ynb])
        P.fence([mergedT], [silu_z])
        for s in range(2):
            wb = load_slab(li, "win", s)
            for pr in range(2):
                items = []
                for t4 in (2 * pr, 2 * pr + 1):
                    tq = s * 4 + t4
                    ps = gbank()
                    gemm_A(wb, t4 * 128, 128, xTb, ps)
                    items.append((ps, 128, 4, halo_qk[li], tq, pl[:, o["mcw"] + tq * 4:o["mcw"] + tq * 4 + 4],
                                  pl[:, o["mcb"] + tq:o["mcb"] + tq + 1]))
                accs = conv_pair(items, pl)
                for it, acc in zip(items, accs):
                    tq = it[4]
                    if tq < 4:
                        memset("pool", qkT[64:128, tq, :], 0.0, [qkT.c(tq)])
                        act(qkT[0:64, tq, :], acc[0:64, 0:512], AF.Silu, [acc], [qkT.c(tq)])
                        act(qz1[64:128, tq, :], acc[64:128, 0:512], AF.Silu, [acc], [qz1.c(tq)])
                    else:
                        act(qkT[:, tq, :], acc[:, 0:512], AF.Silu, [acc], [qkT.c(tq)])
        chk("qk")
        for s in range(4):
            wb = load_slab(li, "win", 2 + s)
            for pr in range(2):
                items = []
                for t4 in (2 * pr, 2 * pr + 1):
                    tq = s * 4 + t4
                    ps = gbank()
                    gemm_A(wb, t4 * 128, 128, xTb, ps)
                    items.append((ps, 128, 4, halo_x[li], tq, pl[:, o["scw"] + tq * 4:o["scw"] + tq * 4 + 4],
                                  pl[:, o["scb"] + tq:o["scb"] + tq + 1]))
                accs = conv_pair(items, pl)
                for it, acc in zip(items, accs):
                    act(xbcT[:, it[4], :], acc[:, 0:512], AF.Silu, [acc], [xbcT.c(it[4])])
        chk("xbc")
        for which in range(3):
            for s in range(2):
                wb = load_slab(li, "win", 10 + 2 * which + s)
                for t4 in range(NTT):
                    ps = gbank()
                    for kt in range(KT):
                        mm(ps[:, :], xTb[:, kt, t4 * 128:(t4 + 1) * 128], wb[:, kt, :], kt == 0, kt == KT - 1,
                           [xTb.c(kt), wb], [ps])
                    if which == 0:
                        cp("act", v_aug[:, t4, 4 * s:4 * s + 4, 0:128], ps[:, :].rearrange("p (h c) -> p h c", h=4),
                           [ps], [v_aug.c(t4)])
                    elif which == 1:
                        act(sig_o[:, t4, s * 512:(s + 1) * 512], ps[:, :], AF.Sigmoid, [ps], [sig_o.c(t4)])
                    else:
                        act(silu_z[:, t4, s * 512:(s + 1) * 512], ps[:, :], AF.Silu, [ps], [silu_z.c(t4)])
        for t4 in range(NTT):
            memset("pool", v_aug[:, t4, :, 128:129], 1.0, [v_aug.c(t4)])
        chk("voz")
        wb = WS[ws_ctr[0] % NWS]
        ws_ctr[0] += 1
        wbf = wb[:].rearrange("p k c -> p (k c)")
        P.dma("sp", wbf[:, 0:256], Sd[li]["winm"].h, reads=[Sd[li]["winm"]], writes=[wb])
        wm = wbf[:, 0:256].rearrange("p (k c) -> p k c", k=KT)
        ps_i, ps_f, ps_dt = PB[3], PB[4], PB[5]
        for (ps_, c0, M) in ((ps_i, 0, 8), (ps_f, 8, 8), (ps_dt, 16, 16)):
            for kt in range(KT):
                mm(ps_[0:M, :], wm[:, kt, c0:c0 + M], xTb[:, kt, :], kt == 0, kt == KT - 1, [wb, xTb.c(kt)], [ps_])
        act(Gdt[:], ps_dt[0:16, :], AF.Exp, [ps_dt, pl], [Gdt], bias=pl[0:16, o["dtb"]:o["dtb"] + 1])
        act(Gdt[:], Gdt[:], AF.Ln, [Gdt], [Gdt], bias=1.0)
        chk("mini")
        for _ in mlstm_gates(li, ps_i, ps_f):
            pass
        if stop != "mgates":
            for _ in ssd_gates(li):
                pass

        for st_ in range(NTT + 1):
            gens = []
            if st_ < NTT:
                gens.append(mlstm_chunk(li, st_))
                if stop not in ("mgates", "mlstm"):
                    gens.append(ssd_chunk(li, st_))
            if st_ >= 1:
                gens.append(mlstm_tail(li, st_ - 1))
                if stop not in ("mgates", "mlstm"):
                    gens.append(ssd_tail(li, st_ - 1))
            while gens:
                for g_ in list(gens):
                    try:
                        next(g_)
                    except StopIteration:
                        gens.remove(g_)
        chk("ssd")
        if "h_aT" in dbg:
            dump("h_aT", h_aT, li)
            dump("h_bT", h_bT, li)
        P.fence([silu_z], [mergedT])
        for br, (pname, gslab, hT) in enumerate((("pa", 6, h_aT), ("pb", 8, h_bT))):
            for half in range(2):
                wp = load_slab(li, pname, half)
                wg = load_slab(li, "win", gslab + half)
                ps1s = []
                for t4 in range(4):
                    ps1 = PB[t4]
                    gemm_A(wp, t4 * 128, 128, hT, ps1)
                    ps1s.append(ps1)
                for t4 in range(4):
                    kt_o = half * 4 + t4
                    ps1 = ps1s[t4]
                    ps2 = PB[4 + t4 % 2]
                    gemm_A(wg, t4 * 128, 128, xTb, ps2)
                    sgb = T4[kt_o % 4]
                    sg = sgb[:, 0:512]
                    act(sg, ps2[:, :], AF.Sigmoid, [ps2], [sgb])
                    if br == 0:
                        tt("dve", mergedT[:, kt_o, :], ps1[:, :], sg, ALU.mult, [ps1, sgb], [mergedT.c(kt_o)])
                    else:
                        tt("dve", sg, ps1[:, :], sg, ALU.mult, [ps1, sgb], [sgb])
                        tt("pool", mergedT[:, kt_o, :], mergedT[:, kt_o, :], sg, ALU.add,
                           [mergedT.c(kt_o), sgb], [mergedT.c(kt_o)])
        chk("merge")
        for half in range(2):
            wb = load_slab(li, "wo", half)
            for t4 in range(4):
                kt_o = half * 4 + t4
                ps = gbank()
                gemm_A(wb, t4 * 128, 128, mergedT, ps)
                stt(xT[:, kt_o, :], xT[:, kt_o, :], ALPHA, ps[:, :], ALU.mult, ALU.add, [xT.c(kt_o), ps],
                    [xT.c(kt_o)])
        layernorm(pl[:, o["ln1g"]:o["ln1g"] + 8], pl[:, o["ln1b"]:o["ln1b"] + 8], pl)
        if "x1" in dbg:
            dump("x1", xT, li)
        chk("ln1")
        P.fence([xbcT, qkT], [hidT])
        P.fence([v_aug, sig_o, hgb, ynb], WDb)
        for s in range(11):
            wb = load_slab(li, "wup", s)
            for jj in range(2):
                j = 2 * s + jj
                M = 128 if j < 21 else 64
                base = jj * 256
                items = []
                for gv in range(2):
                    ps = gbank()
                    gemm_A(wb, base + gv * M, M, xTb, ps)
                    hc = 2 * j + gv
                    wof = o["fcw"] + hc * 3
                    items.append((ps, M, 3, halo_f[li], hc, pl[:, wof:wof + 3], pl[:, o["fcb"] + hc:o["fcb"] + hc + 1]))
                accs = conv_pair(items, pl)
                act(accs[0][0:M, 0:512], accs[0][0:M, 0:512], AF.Silu, [accs[0]], [accs[0]])
                tt("pool", hidT[0:M, j, :], accs[0][0:M, 0:512], accs[1][0:M, 0:512], ALU.mult, [accs[0], accs[1]],
                   [hidT.c(j)])
        chk("ffnup")
        memset("pool", hidT[64:128, NJ - 1, :], 0.0, [hidT.c(NJ - 1)])
        for q in range(8):
            wd = WDb[q % 4]
            P.dma("sp", wd[:].rearrange("p j c -> p (j c)"), Sd[li]["wdn"].h[q], reads=[Sd[li]["wdn"].c(q)],
                  writes=[wd])
            kt_o = q
            ps = gbank()
            for j in range(NJ):
                mm(ps[:, :], wd[:, j, :], hidT[:, j, :], j == 0, j == NJ - 1, [wd, hidT.c(j)], [ps])
            stt(xT[:, kt_o, :], xT[:, kt_o, :], ALPHA, ps[:, :], ALU.mult, ALU.add, [xT.c(kt_o), ps],
                [xT.c(kt_o)])
        layernorm(pl[:, o["ln2g"]:o["ln2g"] + 8], pl[:, o["ln2b"]:o["ln2b"] + 8], pl)

    def dump(name, buf, li):
        key = f"dbg_{name}{li}"
        if key in dbg_out:
            return
        d = nc.dram_tensor(key, [128, KT * TB], buf.h.dtype if hasattr(buf.h, "dtype") else F32,
                           kind="ExternalOutput").ap()
        dbg_out[key] = d
        P.dma("sp", d, buf[:].rearrange("p k t -> p (k t)"), reads=[buf], sem_buf=buf)

    for u in range(n_units):
        if u in seq_starts:
            for li in range(nlayers):
                memset("pool", halo_qk[li][:], 0.0, [halo_qk[li]])
                memset("pool", halo_x[li][:], 0.0, [halo_x[li]])
                memset("pool", halo_f[li][:], 0.0, [halo_f[li]])
                memset("pool", Bc[li][:], 0.0, [Bc[li]])
                memset("pool", Rc[li][:], NEG_BIG, [Rc[li]])
                memset("pool", Sst[li][:], 0.0, [Sst[li]])
                memset("pool", Hst[li][:], 0.0, [Hst[li]])
                memset("pool", Hb[li][:], 0.0, [Hb[li]])
        P.fence([xbcT, qkT, hidT], [xin])
        P.dma("sp", xin[:].rearrange("p k t -> p (k t)"), xT_d[u], writes=[xin])
        if entry_ln:
            layernorm(prm[0][:, _off["inlg"]:_off["inlg"] + 8], prm[0][:, _off["inlb"]:_off["inlb"] + 8], prm[0],
                      src=xin)
        else:
            for kt in range(KT):
                cp("pool", xT[:, kt, :], xin[:, kt, :], [xin.c(kt)], [xT.c(kt)])
                cp("act", xTb[:, kt, :], xin[:, kt, :], [xin.c(kt)], [xTb.c(kt)])
        P.fence([xin], [xbcT, qkT, hidT])
        for li in range(nlayers):
            layer(li)
        P.dma("sp", oT_d[u], xT[:].rearrange("p k t -> p (k t)"), reads=[xT], sem_buf=xT)

    P.emit()
    es.close()
    return nc, list(dbg_out.keys())


def _x_units(x, core):
    units = []
    for s in range(2):
        b = 2 * core + s
        for blk in range(NBLK):
            xb = x[b, blk * TB:(blk + 1) * TB, :]
            a = xb.T.reshape(KT, 128, TB).transpose(1, 0, 2)
            units.append(np.ascontiguousarray(a).reshape(128, KT * TB))
    return np.stack(units)


def _units_to_out(o_units, out, core):
    for s in range(2):
        b = 2 * core + s
        for blk in range(NBLK):
            a = o_units[s * NBLK + blk].reshape(128, KT, TB).transpose(1, 0, 2).reshape(D, TB)
            out[b, blk * TB:(blk + 1) * TB, :] = a.T


_CACHE = {}


def _get_program(key, *args):
    if key not in _CACHE:
        _CACHE[key] = build_program(*args)
    return _CACHE[key]


FUSED = True


def kernel(**inp):
    inp = {k: np.asarray(v, np.float32) for k, v in inp.items()}
    x = inp["x"]
    ncores = 8
    cst = _make_cst()
    lay = [_layer_arrays(inp, l) for l in range(DEPTH)]
    n_units = 2 * NBLK
    seq_starts = {0, NBLK}
    out = np.zeros((BATCH, SEQ, D), np.float32)
    if FUSED:
        nc, _ = _get_program("fused", n_units, seq_starts, DEPTH, True)
        in_maps = []
        for c in range(ncores):
            m = {"xT": _x_units(x, c), "cst": cst}
            for l in range(DEPTH):
                for n, _sh in WSHAPES:
                    m[f"{n}{l}"] = lay[l][n]
                m[f"prm{l}"] = lay[l]["prm"]
                m[f"dexp{l}"] = lay[l]["dexp"]
            in_maps.append(m)
        res = run_bass_kernel_spmd(nc, in_maps, core_ids=list(range(ncores)))
        for c in range(ncores):
            _units_to_out(res.results[c]["oT"], out, c)
        return out
    cur = [_x_units(x, c) for c in range(ncores)]
    for l in range(DEPTH):
        nc, _ = _get_program(("layer", l == 0), n_units, seq_starts, 1, l == 0)
        in_maps = []
        for c in range(ncores):
            m = {"xT": cur[c], "cst": cst}
            for n, _sh in WSHAPES:
                m[f"{n}0"] = lay[l][n]
            m["prm0"] = lay[l]["prm"]
            m["dexp0"] = lay[l]["dexp"]
            in_maps.append(m)
        res = run_bass_kernel_spmd(nc, in_maps, core_ids=list(range(ncores)))
        cur = [np.asarray(res.results[c]["oT"], np.float32) for c in range(ncores)]
    for c in range(ncores):
        _units_to_out(cur[c], out, c)
    return out
```

```python
import numpy as np
from contextlib import ExitStack
import concourse.bass as bass
import concourse.mybir as mybir
from concourse.bass_utils import run_bass_kernel_spmd
from concourse.alu_op_type import AluOpType as ALU

AF = mybir.ActivationFunctionType
AX = mybir.AxisListType
F32 = mybir.dt.float32
BF16 = mybir.dt.bfloat16

DEPTH = 2
D = 1024
KT = 8
SEQ = 2048
BATCH = 16
TB = 512
NTT = 4
NBLK = SEQ // TB
M_H, DQK, DV = 8, 64, 128
S_H, S_P, S_G, S_N = 16, 64, 4, 128
DFF = 2752
NJ = 22
D_IN = 8224
ALPHA = (2 * DEPTH) ** 0.25
LN_EPS = 1e-5
RMS_EPS = 1e-6
NEG_BIG = -1e30

ENGS = ("pe", "act", "dve", "pool", "sp")
RELAX = ()
SCHEDULE = True


class DG:
    def __init__(self, name):
        self.name = name
        self.sem = None
        self.count = 0


class Buf:
    def __init__(self, h, name, ncell=1):
        self.h = h
        self.name = name
        self.ncell = ncell
        self.lw = [None] * ncell
        self.rd = [[] for _ in range(ncell)]
        self.dg = None

    def __getitem__(self, k):
        return self.h[k]

    def c(self, *idx):
        return (self, list(idx))


def _cells(x):
    if isinstance(x, Buf):
        return [(x, i) for i in range(x.ncell)]
    b, idx = x
    return [(b, i) for i in idx]


class Op:
    __slots__ = ("eng", "fn", "idx", "deps", "sig", "signo", "dma", "dg", "dval", "cost", "alld")


class Prog:
    def __init__(self, nc, es):
        self.nc = nc
        self.es = es
        self.ops = []
        self.dgs = []
        self.nsig = {e: 0 for e in ENGS}

    def sbuf(self, name, shape, dtype, ncell=1):
        h = self.es.enter_context(self.nc.sbuf_tensor("t_" + name, list(shape), dtype))
        return Buf(h, name, ncell)

    def psum(self, name, shape, dtype, ncell=1):
        h = self.es.enter_context(self.nc.psum_tensor("t_" + name, list(shape), dtype))
        return Buf(h, name, ncell)

    def op(self, eng, fn, reads=(), writes=(), dma=None, cost=0.5):
        o = Op()
        o.eng, o.fn, o.idx = eng, fn, len(self.ops)
        o.cost = cost
        o.sig, o.signo, o.dma, o.dg, o.dval = False, None, dma is not None, None, None
        deps = {}
        for r in reads:
            for (b, i) in _cells(r):
                if b.lw[i] is not None:
                    deps[b.lw[i]] = True
        for w in writes:
            for (b, i) in _cells(w):
                if b.lw[i] is not None:
                    deps.setdefault(b.lw[i], False)
                for j in b.rd[i]:
                    deps.setdefault(j, False)
        deps.pop(o.idx, None)
        o.deps = deps
        for r in reads:
            for (b, i) in _cells(r):
                b.rd[i].append(o.idx)
        for w in writes:
            for (b, i) in _cells(w):
                b.lw[i] = o.idx
                b.rd[i] = []
        if o.dma:
            if dma.dg is None:
                dma.dg = DG(dma.name)
                self.dgs.append(dma.dg)
            o.dg = dma.dg
            o.dg.count += 16
            o.dval = o.dg.count
        self.ops.append(o)
        return o

    def dma(self, eng, out_ap, in_ap, reads=(), writes=(), sem_buf=None):
        if sem_buf is None:
            sem_buf = _cells(writes[0])[0][0] if writes else _cells(reads[0])[0][0]
        nbytes = 1
        for d_ in in_ap.shape:
            nbytes *= d_
        nbytes *= 4 if in_ap.dtype == F32 else 2
        return self.op(eng, lambda e: e.dma_start(out=out_ap, in_=in_ap),
                       reads=reads, writes=writes, dma=sem_buf, cost=1.5 + nbytes / 200e3)

    def fence(self, old, new):
        s = set()
        for b in old:
            for i in range(b.ncell):
                if b.lw[i] is not None:
                    s.add(b.lw[i])
                s.update(b.rd[i])
        for b in new:
            for i in range(b.ncell):
                b.rd[i].extend(s)

    def schedule(self):
        import heapq
        ops = self.ops
        n = len(ops)
        succ = [[] for _ in range(n)]
        indeg = [0] * n
        for o in ops:
            for j in o.deps:
                succ[j].append(o.idx)
                indeg[o.idx] += 1
        ready_t = [0.0] * n
        fin = [0.0] * n
        heaps = {e: [] for e in ENGS}
        free = {e: 0.0 for e in ENGS}
        for o in ops:
            if indeg[o.idx] == 0:
                heapq.heappush(heaps[o.eng], o.idx)
        out = []
        LAT = 0.2
        WIN = 48
        while len(out) < n:
            best = None
            for e in ENGS:
                h = heaps[e]
                if not h:
                    continue
                cands = heapq.nsmallest(WIN, h)
                bi, bs = None, None
                for i in cands:
                    st = max(ready_t[i], free[e])
                    if bs is None or st < bs - 1e-9:
                        bi, bs = i, st
                if best is None or bs < best[0] - 1e-9 or (abs(bs - best[0]) <= 1e-9 and bi < best[1]):
                    best = (bs, bi, e)
            bs, bi, e = best
            heaps[e].remove(bi)
            heapq.heapify(heaps[e])
            o = ops[bi]
            f = bs + o.cost
            free[e] = bs + (0.15 if o.dma else o.cost)
            fin[bi] = f
            out.append(o)
            for k in succ[bi]:
                ready_t[k] = max(ready_t[k], f + LAT)
                indeg[k] -= 1
                if indeg[k] == 0:
                    heapq.heappush(heaps[ops[k].eng], k)
        self.sim_time = max(fin) if fin else 0.0
        return out

    def emit(self):
        nc, es, ops = self.nc, self.es, self.ops
        sched = self.schedule() if SCHEDULE else None
        for o in ops:
            need = []
            for j, raw in o.deps.items():
                p = ops[j]
                if p.dma:
                    need.append(j)
                    continue
                if p.eng == o.eng and not o.dma:
                    if o.eng == "pe" or (not raw and o.eng in RELAX):
                        continue
                need.append(j)
            o.deps = need
            for j in need:
                if not ops[j].dma:
                    ops[j].sig = True
        for o in (sched if sched is not None else ops):
            if o.sig and not o.dma:
                self.nsig[o.eng] += 1
                o.signo = self.nsig[o.eng]
        esem = {e: es.enter_context(nc.semaphore("s_" + e)) for e in ENGS}
        for g in self.dgs:
            g.sem = es.enter_context(nc.semaphore("d_" + g.name))
        streams = {e: [] for e in ENGS}
        for o in (sched if sched is not None else ops):
            streams[o.eng].append(o)
        block = es.enter_context(nc.Block())
        prog = self

        def run(eng_name, engine):
            seen = {e: 0 for e in ENGS}
            dseen = {}
            for o in streams[eng_name]:
                wl, dl = {}, {}
                for j in o.deps:
                    p = ops[j]
                    if p.dma:
                        k = id(p.dg)
                        if dseen.get(k, 0) < p.dval and (k not in dl or dl[k][1] < p.dval):
                            dl[k] = (p.dg, p.dval)
                    elif seen[p.eng] < p.signo:
                        wl[p.eng] = max(wl.get(p.eng, 0), p.signo)
                for e, v in wl.items():
                    engine.wait_ge(esem[e], v)
                    seen[e] = v
                for k, (g, v) in dl.items():
                    engine.wait_ge(g.sem, v)
                    dseen[k] = v
                ins = o.fn(engine)
                if o.dma:
                    ins.then_inc(o.dg.sem, 16)
                elif o.sig:
                    ins.then_inc(esem[eng_name], 1)
            if eng_name == "sp":
                for e in ENGS:
                    if prog.nsig[e]:
                        engine.wait_ge(esem[e], prog.nsig[e])
                for g in prog.dgs:
                    engine.wait_ge(g.sem, g.count)

        @block.tensor
        def _(e):
            run("pe", e)

        @block.scalar
        def _(e):
            run("act", e)

        @block.vector
        def _(e):
            run("dve", e)

        @block.gpsimd
        def _(e):
            run("pool", e)

        @block.sync
        def _(e):
            run("sp", e)


C_ID, C_M01, C_NEG, C_ONE, C_BLK, C_RST, C_SEL, C_O16 = 0, 128, 256, 384, 512, 528, 1040, 1552
NC_CST = 1552 + 512

_off = {}
_o = 0
for _n, _w in (("mcw", 32), ("mcb", 8), ("scw", 64), ("scb", 16), ("fcw", 132), ("fcb", 44),
               ("ln1g", 8), ("ln1b", 8), ("ln2g", 8), ("ln2b", 8), ("mng", 8), ("sng", 8),
               ("ib", 1), ("fb", 1), ("dtb", 1), ("alog", 1), ("inlg", 8), ("inlb", 8)):
    _off[_n] = _o
    _o += _w
NPRM = _o

Q0, K0, V0, O0, I0, F0, Z0, X0, DT0, GA0, GB0 = 0, 512, 1024, 2048, 3072, 3080, 3088, 4112, 6160, 6176, 7200


def _slab(wcols):
    n = wcols.shape[1] // 512
    a = wcols.reshape(KT, 128, n, 512).transpose(2, 1, 0, 3)
    return np.ascontiguousarray(a).reshape(n, 128, KT * 512)


def _pt(v, ntile):
    return np.ascontiguousarray(v.reshape(ntile, 128).T)


def _make_cst():
    c = np.zeros((128, NC_CST), np.float32)
    c[:, C_ID:C_ID + 128] = np.eye(128, dtype=np.float32)
    s = np.arange(128)[:, None]
    t = np.arange(128)[None, :]
    c[:, C_M01:C_M01 + 128] = (s <= t).astype(np.float32)
    c[:, C_NEG:C_NEG + 128] = np.where(s <= t, 0.0, -30000.0).astype(np.float32)
    c[:, C_ONE:C_ONE + 128] = 1.0
    c[0:16, C_BLK:C_BLK + 16] = np.eye(16, dtype=np.float32)
    r = np.ones((16, 512), np.float32)
    r[:, 0::128] = 0.0
    c[0:16, C_RST:C_RST + 512] = r
    sel = np.zeros((8, 4, 128), np.float32)
    for hp in range(4):
        sel[2 * hp, hp, 0:64] = 1.0
        sel[2 * hp + 1, hp, 64:128] = 1.0
    c[0:8, C_SEL:C_SEL + 512] = sel.reshape(8, 512)
    c[0:16, C_O16:C_O16 + 512] = 1.0
    return c


def _layer_arrays(inp, l):
    f32 = np.float32
    w_in = inp["w_in"][l]
    out = {}
    cols = [w_in[:, Q0:Q0 + 1024], w_in[:, X0:X0 + 2048], w_in[:, GA0:GA0 + 1024], w_in[:, GB0:GB0 + 1024],
            w_in[:, V0:V0 + 1024], w_in[:, O0:O0 + 1024], w_in[:, Z0:Z0 + 1024]]
    out["win"] = _slab(np.concatenate(cols, axis=1))
    mini = np.concatenate([w_in[:, I0:I0 + 8], w_in[:, F0:F0 + 8], w_in[:, DT0:DT0 + 16]], axis=1)
    out["winm"] = np.ascontiguousarray(mini.reshape(KT, 128, 32).transpose(1, 0, 2)).reshape(128, 256)
    out["pa"] = _slab(inp["p_a"][l])
    out["pb"] = _slab(inp["p_b"][l])
    out["wo"] = _slab(inp["w_out"][l])
    w_up = inp["w_up"][l]
    perm = []
    for j in range(NJ):
        m = 128 if j < 21 else 64
        perm += list(range(128 * j, 128 * j + m)) + list(range(DFF + 128 * j, DFF + 128 * j + m))
    wup_p = np.zeros((1024, 11 * 512), f32)
    wup_p[:, :len(perm)] = w_up[:, perm]
    out["wup"] = _slab(wup_p)
    wd = np.zeros((NJ * 128, 1024), f32)
    wd[:DFF] = inp["w_down"][l]
    out["wdn"] = np.ascontiguousarray(wd.reshape(NJ, 128, 8, 128).transpose(2, 1, 0, 3)).reshape(8, 128, NJ * 128)
    prm = np.zeros((128, NPRM), f32)

    def put(name, arr):
        a = np.asarray(arr, f32)
        prm[:a.shape[0], _off[name]:_off[name] + a.shape[1]] = a

    mcw = inp["m_conv_w"][l]
    put("mcw", mcw.T.reshape(8, 128, 4).transpose(1, 0, 2).reshape(128, 32))
    put("mcb", _pt(inp["m_conv_b"][l], 8))
    scw = inp["s_conv_w"][l]
    put("scw", scw.T.reshape(16, 128, 4).transpose(1, 0, 2).reshape(128, 64))
    put("scb", _pt(inp["s_conv_b"][l], 16))
    fcw = inp["f_conv_w"][l]
    fcb = inp["f_conv_b"][l]
    fw = np.zeros((128, NJ, 2, 3), f32)
    fb = np.zeros((128, NJ, 2), f32)
    for j in range(NJ):
        m = 128 if j < 21 else 64
        fw[:m, j, 0, :] = fcw[:, 128 * j:128 * j + m].T
        fw[:m, j, 1, :] = fcw[:, DFF + 128 * j:DFF + 128 * j + m].T
        fb[:m, j, 0] = fcb[128 * j:128 * j + m]
        fb[:m, j, 1] = fcb[DFF + 128 * j:DFF + 128 * j + m]
    put("fcw", fw.reshape(128, 132))
    put("fcb", fb.reshape(128, 44))
    put("ln1g", _pt(inp["ln1_g"][l], 8))
    put("ln1b", _pt(inp["ln1_b"][l], 8))
    put("ln2g", _pt(inp["ln2_g"][l], 8))
    put("ln2b", _pt(inp["ln2_b"][l], 8))
    put("mng", _pt(inp["m_norm_g"][l], 8))
    put("sng", _pt(inp["s_norm_g"][l], 8))
    put("ib", inp["m_i_bias"][l].reshape(8, 1))
    put("fb", inp["m_f_bias"][l].reshape(8, 1))
    put("dtb", inp["s_dt_bias"][l].reshape(16, 1))
    put("alog", inp["s_a_log"][l].reshape(16, 1))
    put("inlg", _pt(inp["in_ln_g"], 8))
    put("inlb", _pt(inp["in_ln_b"], 8))
    out["prm"] = prm
    out["dexp"] = np.ascontiguousarray(np.repeat(inp["s_d"][l], S_P).reshape(1, 1024).astype(f32))
    return out


WSHAPES = (("win", [16, 128, 4096]), ("winm", [128, 256]), ("pa", [2, 128, 4096]), ("pb", [2, 128, 4096]),
           ("wo", [2, 128, 4096]), ("wup", [11, 128, 4096]), ("wdn", [8, 128, NJ * 128]))


def build_program(n_units, seq_starts, nlayers, entry_ln, dbg=None, stop=None):
    nc = bass.Bass("TRN2", target_bir_lowering=False)
    es = ExitStack()
    P = Prog(nc, es)
    dbg = dbg or {}
    dbg_out = {}

    def din(name, shape):
        return nc.dram_tensor(name, list(shape), F32, kind="ExternalInput").ap()

    xT_d = din("xT", [n_units, 128, KT * TB])
    oT_d = nc.dram_tensor("oT", [n_units, 128, KT * TB], F32, kind="ExternalOutput").ap()
    cst_d = din("cst", [128, NC_CST])
    Wd, Sd = [], []
    for li in range(nlayers):
        w = {n: din(f"{n}{li}", sh) for n, sh in WSHAPES}
        w["prm"] = din(f"prm{li}", [128, NPRM])
        w["dexp"] = din(f"dexp{li}", [1, 1024])
        Wd.append(w)
        s = {}
        for n, sh in WSHAPES:
            ncell = sh[0] if len(sh) == 3 else 1
            s[n] = Buf(nc.dram_tensor(f"s_{n}{li}", list(sh), BF16).ap(), f"s_{n}{li}", ncell)
        Sd.append(s)

    xT = P.sbuf("xT", [128, KT, TB], F32, KT)
    xTb = P.sbuf("xTb", [128, KT, TB], BF16, KT)
    NWS = 3
    WS = [P.sbuf(f"ws{i}", [128, KT, 512], BF16) for i in range(NWS)]
    ar1 = es.enter_context(nc.sbuf_tensor("ar1", [128, 12288], BF16))
    ar2 = es.enter_context(nc.sbuf_tensor("ar2", [128, 11264], BF16))
    xbcT = Buf(ar1[:, 0:8192].rearrange("p (k t) -> p k t", k=16), "xbcT", 16)
    qkT = Buf(ar1[:, 8192:12288].rearrange("p (k t) -> p k t", k=8), "qkT", 8)
    hidT = Buf(ar1[:, 0:11264].rearrange("p (k t) -> p k t", k=NJ), "hidT", NJ)
    stg = [Buf(ar1[:, 0:8192].bitcast(F32), "stg0"), Buf(ar2[:, 0:8192].bitcast(F32), "stg1")]
    xin = Buf(ar1[:, 0:8192].bitcast(F32).rearrange("p (k t) -> p k t", k=KT), "xin", KT)
    WDb = [Buf(ar2[:, i * 2816:(i + 1) * 2816].rearrange("p (j c) -> p j c", j=NJ), f"wd{i}") for i in range(4)]
    v_aug = Buf(ar2[:, 0:4160].rearrange("p (t h c) -> p t h c", t=NTT, h=M_H), "v_aug", NTT)
    sig_o = Buf(ar2[:, 4160:8256].rearrange("p (t c) -> p t c", t=NTT), "sig_o", NTT)
    silu_z = P.sbuf("silu_z", [128, NTT, 1024], BF16, NTT)
    h_aT = P.sbuf("h_aT", [128, KT, TB], BF16, KT)
    h_bT = P.sbuf("h_bT", [128, KT, TB], BF16, KT)
    mergedT = Buf(silu_z.h[:].rearrange("p t c -> p (t c)").rearrange("p (k t) -> p k t", k=KT), "mergedT", KT)
    T4 = [P.sbuf(f"t4_{i}", [128, 1040], F32) for i in range(4)]
    cst = P.sbuf("cst", [128, NC_CST], F32)
    ident_b = P.sbuf("ident_b", [128, 128], BF16)
    ones_b = P.sbuf("ones_b", [128, 128], BF16)
    negm4 = P.sbuf("negm4", [128, 512], BF16)
    mask_m = P.sbuf("mask_m", [128, 128], F32)
    prm = [P.sbuf(f"prm{li}", [128, NPRM], F32) for li in range(nlayers)]
    nfb = [P.sbuf(f"nfb{li}", [8, 1], F32) for li in range(nlayers)]
    negA = [P.sbuf(f"negA{li}", [16, 1], F32) for li in range(nlayers)]
    halo_qk = [P.sbuf(f"hqk{li}", [128, 8, 3], F32, 8) for li in range(nlayers)]
    halo_x = [P.sbuf(f"hx{li}", [128, 16, 3], F32, 16) for li in range(nlayers)]
    halo_f = [P.sbuf(f"hf{li}", [128, 2 * NJ, 2], F32, 2 * NJ) for li in range(nlayers)]
    Bc = [P.sbuf(f"bc{li}", [8, 1], F32) for li in range(nlayers)]
    Rc = [P.sbuf(f"rc{li}", [8, 1], F32) for li in range(nlayers)]
    Sst = [P.sbuf(f"sst{li}", [128, 4, 130], F32) for li in range(nlayers)]
    Hst = [P.sbuf(f"hst{li}", [128, 16, 64], F32) for li in range(nlayers)]
    Hb = [P.sbuf(f"hb{li}", [128, 16, 64], BF16) for li in range(nlayers)]
    Sb = P.sbuf("sb", [128, 4, 130], BF16)
    G = [P.sbuf(f"g{i}", [16, 512], F32) for i in range(4)]
    Gdt = P.sbuf("gdt", [16, 512], F32)
    sm_cm = P.sbuf("sm_cm", [16, 4], F32)
    sm_R = P.sbuf("sm_R", [16, 4], F32)
    sm_Rp = P.sbuf("sm_Rp", [16, 4], F32)
    sm_gm = P.sbuf("sm_gm", [16, 4], F32)
    sm_cd = P.sbuf("sm_cd", [16, 4], F32)
    sm_dg = P.sbuf("sm_dg", [16, 4, 16], F32)
    gtok = P.sbuf("gtok", [128, NTT, 16], F32)
    gam_bc = P.sbuf("gam_bc", [128, 4, 4], F32)
    stok = P.sbuf("stok", [128, NTT, 64], F32)
    cd_bc = P.sbuf("cd_bc", [128, 4, 16], F32)
    k_tok = P.sbuf("k_tok", [128, 512], BF16)
    qz1 = P.sbuf("qz1", [128, 4, 512], BF16, 4)
    vw = P.sbuf("vw", [128, 8, 130], BF16)
    PT = [P.sbuf(f"pt{i}", [128, 2, 128], BF16) for i in range(2)]
    sm_d = P.sbuf("sm_d", [128, 8], F32)
    sm_ss = P.sbuf("sm_ss", [128, 8], F32)
    sm_ss2 = P.sbuf("sm_ss2", [128, 8], F32)
    junk = P.sbuf("junk", [128, 128], BF16)
    junk2 = P.sbuf("junk2", [128, 256], BF16)
    hgb = Buf(ar2[:, 8256:9280], "hgb")
    ynb = Buf(ar2[:, 9280:10304], "ynb")
    Dm = [P.sbuf(f"dm{li}", [128, 16, 64], BF16) for li in range(nlayers)]
    xs_tok = P.sbuf("xs_tok", [128, 1024], BF16)
    B_tok = P.sbuf("B_tok", [128, 512], BF16)
    xdt = P.sbuf("xdt", [128, 16, 64], BF16)
    xdte = P.sbuf("xdte", [128, 16, 64], BF16)
    CBm = P.sbuf("CBm", [128, 4, 128], F32)
    Zg = [P.sbuf(f"zg{i}", [128, 4, 128], F32) for i in range(2)]
    decb = [P.sbuf(f"dec{i}", [128, 4, 128], BF16) for i in range(2)]
    Gg = [P.sbuf(f"gg{i}", [128, 4, 128], BF16) for i in range(2)]
    PB = [P.psum(f"pb{i}", [128, 512], F32) for i in range(7)]
    PBT = P.psum("pbt", [128, 1024], BF16)

    ident_f = cst[:, C_ID:C_ID + 128]
    mask01 = cst[:, C_M01:C_M01 + 128]
    ones_f = cst[:, C_ONE:C_ONE + 128]

    def fsz(ap):
        n = 1
        for d_ in ap.shape[1:]:
            n *= d_
        return n

    def act(out, in_, func, reads, writes, bias=None, scale=None):
        kw = {}
        if bias is not None:
            kw["bias"] = bias
        if scale is not None:
            kw["scale"] = scale
        P.op("act", lambda e: e.activation(out=out, in_=in_, func=func, **kw), reads, writes,
             cost=0.22 + fsz(out) / 1400.0)

    def ecost(eng, out, mult=1.0):
        if eng == "pool":
            return 0.4 + mult * fsz(out) / 520.0
        if eng == "act":
            return 0.22 + mult * fsz(out) / 1400.0
        return 0.12 + mult * fsz(out) / 960.0

    def tt(eng, out, in0, in1, op, reads, writes):
        m_ = 1.0
        if eng == "dve" and not any("t_pb" in a.name for a in (in0, in1)):
            m_ = 2.0
        P.op(eng, lambda e: e.tensor_tensor(out=out, in0=in0, in1=in1, op=op), reads, writes,
             cost=ecost(eng, out, m_))

    def ts(eng, out, in0, s1, op0, reads, writes, s2=None, op1=None):
        if op1 is None:
            P.op(eng, lambda e: e.tensor_scalar(out=out, in0=in0, scalar1=s1, scalar2=None, op0=op0), reads, writes,
                 cost=ecost(eng, out))
        else:
            P.op(eng, lambda e: e.tensor_scalar(out=out, in0=in0, scalar1=s1, scalar2=s2, op0=op0, op1=op1),
                 reads, writes, cost=ecost(eng, out))

    def stt(out, in0, scalar, in1, op0, op1, reads, writes):
        P.op("dve", lambda e: e.scalar_tensor_tensor(out=out, in0=in0, scalar=scalar, in1=in1, op0=op0, op1=op1),
             reads, writes, cost=ecost("dve", out))

    def cp(eng, out, in_, reads, writes):
        if eng == "act":
            P.op("act", lambda e: e.copy(out=out, in_=in_), reads, writes, cost=ecost("act", out))
        else:
            P.op(eng, lambda e: e.tensor_copy(out=out, in_=in_), reads, writes, cost=ecost(eng, out))

    def mm(out, lhsT, rhs, start, stop, reads, writes):
        nn = max(fsz(rhs), 64) / 2000.0
        if rhs.dtype == F32:
            nn *= 4
        P.op("pe", lambda e: e.matmul(out, lhsT=lhsT, rhs=rhs, start=start, stop=stop), reads, writes,
             cost=nn + 0.03)

    def tr(out, in_, ident, reads, writes):
        P.op("pe", lambda e: e.transpose(out, in_, ident), reads, writes, cost=0.09)

    def memset(eng, ap, val, writes):
        P.op(eng, lambda e: e.memset(ap, val), (), writes, cost=ecost(eng, ap))

    def recip(out, in_, reads, writes):
        P.op("dve", lambda e: e.reciprocal(out=out, in_=in_), reads, writes, cost=ecost("dve", out, 8.0))

    P.dma("sp", cst[:], cst_d, writes=[cst])
    for li in range(nlayers):
        P.dma("sp", prm[li][:], Wd[li]["prm"], writes=[prm[li]])
    cp("dve", ident_b[:], ident_f, [cst], [ident_b])
    cp("dve", ones_b[:], ones_f, [cst], [ones_b])
    for q in range(4):
        cp("dve", negm4[:, q * 128:(q + 1) * 128], cst[:, C_NEG:C_NEG + 128], [cst], [negm4])
    ts("dve", mask_m[:], mask01, DQK ** -0.5, ALU.mult, [cst], [mask_m])
    for li in range(nlayers):
        o = _off
        ts("dve", nfb[li][:], prm[li][0:8, o["fb"]:o["fb"] + 1], -1.0, ALU.mult, [prm[li]], [nfb[li]])
        act(negA[li][:], prm[li][0:16, o["alog"]:o["alog"] + 1], AF.Exp, [prm[li]], [negA[li]])
        ts("dve", negA[li][:], negA[li][:], -1.0, ALU.mult, [negA[li]], [negA[li]])
    for li in range(nlayers):
        dexp1 = T4[3]
        P.dma("sp", dexp1[:, 0:1024], Wd[li]["dexp"].partition_broadcast(128), writes=[dexp1])
        for h in range(16):
            e2 = h % 2
            ts("dve", Dm[li][:, h, :], cst[:, C_ID + 64 * e2:C_ID + 64 * e2 + 64], dexp1[:, h * 64:h * 64 + 1],
               ALU.mult, [cst, dexp1], [Dm[li]])
    memset("pool", vw[:], 0.0, [vw])
    for i in range(4):
        memset("pool", T4[i][:], 0.0, [T4[i]])
    for i in range(4):
        memset("pool", G[i][:], 0.0, [G[i]])
    for i in range(2):
        memset("pool", Zg[i][:], 0.0, [Zg[i]])
    memset("pool", qz1[:], 0.0, [qz1])

    jobs = []
    for li in range(nlayers):
        for n, sh in WSHAPES:
            if n == "winm":
                jobs.append((Wd[li][n], Sd[li][n].h, [Sd[li][n]], 256, None, li))
            elif n == "wdn":
                for s in range(sh[0]):
                    jobs.append((Wd[li][n][s], Sd[li][n].h[s], [Sd[li][n].c(s)], 2816, None, li))
            else:
                for s in range(sh[0]):
                    sc = {"pa": "mng", "pb": "sng"}.get(n)
                    jobs.append((Wd[li][n][s], Sd[li][n].h[s], [Sd[li][n].c(s)], 4096, sc, li))
    cast_engs = ("act", "dve", "act")

    def pre_load(ji):
        src, dst, cells, F, sc, li = jobs[ji]
        st = stg[ji % 2]
        P.dma("sp", st[:, 0:F], src, writes=[st])

    def pre_cast_store(ji):
        src, dst, cells, F, sc, li = jobs[ji]
        st = stg[ji % 2]
        wb = WS[ji % NWS]
        wflat = wb[:].rearrange("p k c -> p (k c)")
        if sc is not None:
            g = prm[li][:, _off[sc]:_off[sc] + 8]
            tt("dve", wb[:], st[:, 0:4096].rearrange("p (k c) -> p k c", k=KT),
               g.unsqueeze(2).to_broadcast([128, KT, 512]), ALU.mult, [st, prm[li]], [wb])
        else:
            cp(cast_engs[ji % 3], wflat[:, 0:F], st[:, 0:F], [st], [wb])
        P.dma("sp", dst, wflat[:, 0:F], reads=[wb], writes=cells, sem_buf=wb)

    for ji in range(len(jobs) + 1):
        if ji < len(jobs):
            pre_load(ji)
        if ji >= 1:
            pre_cast_store(ji - 1)
    P.fence(stg, [xbcT, qkT, hidT, xin] + WDb + [v_aug, sig_o, hgb, ynb])

    ws_ctr = [0]

    def load_slab(li, name, s):
        wb = WS[ws_ctr[0] % NWS]
        ws_ctr[0] += 1
        sb = Sd[li][name]
        P.dma("sp", wb[:].rearrange("p k c -> p (k c)"), sb.h[s], reads=[sb.c(s)], writes=[wb])
        return wb

    gb_ctr = [0]

    def gbank():
        b = PB[gb_ctr[0] % 6]
        gb_ctr[0] += 1
        return b

    def layernorm(g_ap, b_ap, pbuf, src=None):
        src = xT if src is None else src
        ps_m, ps_q = PB[5], PB[6]
        for kt in range(KT):
            tb = T4[1 + 2 * (kt % 2)]
            tbv = tb[:].bitcast(BF16)
            act(tbv[:, 0:512], src[:, kt, :], AF.Copy, [src.c(kt)], [tb])
            act(tbv[:, 512:1024], src[:, kt, :], AF.Square, [src.c(kt)], [tb])
            mm(ps_m[:, :], ones_b[:], tbv[:, 0:512], kt == 0, kt == KT - 1, [ones_b, tb], [ps_m])
            mm(ps_q[:, :], ones_b[:], tbv[:, 512:1024], kt == 0, kt == KT - 1, [ones_b, tb], [ps_q])
        mean, var = T4[0], T4[2]
        act(mean[:, 0:512], ps_m[:, :], AF.Copy, [ps_m], [mean], scale=1.0 / D)
        tt("dve", var[:, 0:512], mean[:, 0:512], mean[:, 0:512], ALU.mult, [mean], [var])
        stt(var[:, 0:512], ps_q[:, :], 1.0 / D, var[:, 0:512], ALU.mult, ALU.subtract, [ps_q, var], [var])
        ts("dve", var[:, 0:512], var[:, 0:512], LN_EPS, ALU.add, [var], [var])
        act(var[:, 0:512], var[:, 0:512], AF.Ln, [var], [var])
        act(var[:, 0:512], var[:, 0:512], AF.Exp, [var], [var], scale=-0.5)
        for kt in range(KT):
            tmp = T4[1 + 2 * (kt % 2)]
            tt("pool", tmp[:, 0:512], src[:, kt, :], mean[:, 0:512], ALU.subtract, [src.c(kt), mean], [tmp])
            tt("dve", tmp[:, 0:512], tmp[:, 0:512], var[:, 0:512], ALU.mult, [tmp, var], [tmp])
            act(xT[:, kt, :], tmp[:, 0:512], AF.Identity, [tmp, pbuf], [xT.c(kt)],
                bias=b_ap[:, kt:kt + 1], scale=g_ap[:, kt:kt + 1])
            cp("dve", xTb[:, kt, :], xT[:, kt, :], [xT.c(kt)], [xTb.c(kt)])

    acc_ctr = [0]

    def conv_pair(items, pbuf):
        accs = []
        for i, (ps, M, Kc, halo_buf, hcell, w_ap, b_ap) in enumerate(items):
            acc = T4[acc_ctr[0] % 4]
            acc_ctr[0] += 1
            Hh = Kc - 1
            act(acc[0:M, 0:512], ps[0:M, :], AF.Identity, [ps, pbuf], [acc],
                bias=b_ap[0:M, 0:1], scale=w_ap[0:M, Hh:Hh + 1])
            accs.append(acc)
        Kc = items[0][2]
        Hh = Kc - 1
        for k in range(Kc - 1):
            sh = Hh - k
            for i, (ps, M, _k, halo_buf, hcell, w_ap, b_ap) in enumerate(items):
                acc = accs[i]
                stt(acc[0:M, sh:512], ps[0:M, 0:512 - sh], w_ap[0:M, k:k + 1], acc[0:M, sh:512], ALU.mult, ALU.add,
                    [ps, pbuf, acc], [acc])
                stt(acc[0:M, 0:sh], halo_buf[0:M, hcell, k:Hh], w_ap[0:M, k:k + 1], acc[0:M, 0:sh], ALU.mult,
                    ALU.add, [halo_buf.c(hcell), pbuf, acc], [acc])
        for i, (ps, M, Kc_, halo_buf, hcell, w_ap, b_ap) in enumerate(items):
            cp("act", halo_buf[0:M, hcell, 0:Hh], ps[0:M, 512 - Hh:512], [ps], [halo_buf.c(hcell)])
        return accs

    def gemm_A(wb, c0, M, rhsbuf, ps):
        for kt in range(KT):
            mm(ps[0:M, :], wb[:, kt, c0:c0 + M], rhsbuf[:, kt, :], kt == 0, kt == KT - 1,
               [wb, rhsbuf.c(kt)], [ps])

    def mlstm_gates(li, ps_i, ps_f):
        o = _off
        pl = prm[li]
        act(G[0][0:8, :], ps_f[0:8, :], AF.Exp, [ps_f, nfb[li]], [G[0]], bias=nfb[li][:], scale=-1.0)
        act(G[0][0:8, :], G[0][0:8, :], AF.Ln, [G[0]], [G[0]], bias=1.0)
        P.op("dve", lambda e: e.tensor_tensor_scan(out=G[1][0:8, :], data0=cst[0:8, C_O16:C_O16 + 512],
                                                   data1=G[0][0:8, :], initial=Bc[li][:],
                                                   op0=ALU.mult, op1=ALU.subtract),
             [cst, G[0], Bc[li]], [G[1]])
        cp("pool", Bc[li][:], G[1][0:8, 511:512], [G[1]], [Bc[li]])
        yield
        stt(G[2][0:8, :], ps_i[0:8, :], pl[0:8, o["ib"]:o["ib"] + 1], G[1][0:8, :], ALU.add, ALU.subtract,
            [ps_i, pl, G[1]], [G[2]])
        P.op("dve", lambda e: e.tensor_reduce(out=sm_cm[0:8, :], in_=G[2][0:8, :].rearrange("p (c t) -> p c t", c=4),
                                              axis=AX.X, op=ALU.max), [G[2]], [sm_cm])
        P.op("dve", lambda e: e.tensor_tensor_scan(out=sm_R[0:8, :], data0=sm_cm[0:8, :], data1=sm_cm[0:8, :],
                                                   initial=Rc[li][:], op0=ALU.max, op1=ALU.max),
             [sm_cm, Rc[li]], [sm_R])
        cp("pool", sm_Rp[0:8, 0:1], Rc[li][:], [Rc[li]], [sm_Rp])
        cp("pool", sm_Rp[0:8, 1:4], sm_R[0:8, 0:3], [sm_R], [sm_Rp])
        cp("pool", Rc[li][:], sm_R[0:8, 3:4], [sm_R], [Rc[li]])
        yield
        tt("dve", sm_gm[0:8, :], sm_Rp[0:8, :], sm_R[0:8, :], ALU.subtract, [sm_Rp, sm_R], [sm_gm])
        act(sm_gm[0:8, :], sm_gm[0:8, :], AF.Exp, [sm_gm], [sm_gm])
        Rb = sm_R[0:8, :].unsqueeze(2).to_broadcast([8, 4, 128])
        g2v = G[2][0:8, :].rearrange("p (c t) -> p c t", c=4)
        g1v = G[1][0:8, :].rearrange("p (c t) -> p c t", c=4)
        g3v = G[3][0:8, :].rearrange("p (c t) -> p c t", c=4)
        tt("dve", g2v, g2v, Rb, ALU.subtract, [G[2], sm_R], [G[2]])
        act(G[2][0:8, :], G[2][0:8, :], AF.Exp, [G[2]], [G[2]])
        yield
        tt("dve", g3v, g1v, Rb, ALU.add, [G[1], sm_R], [G[3]])
        act(G[3][0:8, :], G[3][0:8, :], AF.Exp, [G[3]], [G[3]], scale=-1.0)
        yield
        ps_g = PB[2]
        for t4 in range(NTT):
            tsl = slice(t4 * 128, (t4 + 1) * 128)
            tr(ps_g[:, t4 * 16:t4 * 16 + 8], G[2][0:8, tsl], cst[0:8, C_ID:C_ID + 8], [G[2], cst], [ps_g])
            tr(ps_g[:, t4 * 16 + 8:t4 * 16 + 16], G[3][0:8, tsl], cst[0:8, C_ID:C_ID + 8], [G[3], cst], [ps_g])
        for hp in range(4):
            mm(ps_g[:, 64 + hp * 4:64 + hp * 4 + 4], cst[0:8, C_SEL + hp * 128:C_SEL + (hp + 1) * 128],
               sm_gm[0:8, :], True, True, [cst, sm_gm], [ps_g])
        cp("act", gtok[:].rearrange("p t c -> p (t c)"), ps_g[:, 0:64], [ps_g], [gtok])
        cp("act", gam_bc[:].rearrange("p h c -> p (h c)"), ps_g[:, 64:80], [ps_g], [gam_bc])
        yield

    import os as _os
    mstop = int(_os.environ.get("MSTOP", "0"))

    def mchk(k):
        if mstop == k:
            raise _Stop()

    def mlstm_chunk(li, c):
        tsl = slice(c * 128, (c + 1) * 128)
        S = Sst[li]
        for hp in range(4):
            tr(PBT[:, hp * 128:(hp + 1) * 128], qkT[:, 4 + hp, tsl], ident_b[:], [qkT.c(4 + hp), ident_b], [PBT])
        cp("act", k_tok[:], PBT[:, 0:512], [PBT], [k_tok])
        mchk(1)
        yield
        tt("pool", vw[:, :, 0:129], v_aug[:, c, :, 0:129],
           gtok[:, c, 0:8].unsqueeze(2).to_broadcast([128, 8, 129]), ALU.mult, [v_aug.c(c), gtok], [vw])
        tt("pool", S[:, :, 0:129], S[:, :, 0:129],
           gam_bc[:, :, c:c + 1].to_broadcast([128, 4, 129]), ALU.mult, [S, gam_bc], [S])
        act(Sb[:, :, 0:129], S[:, :, 0:129], AF.Copy, [S], [Sb], scale=DQK ** -0.5)
        osb = T4[0][:, 0:1040].rearrange("p (h c) -> p h c", h=8)
        mchk(2)
        yield
        for hp in range(4):
            X, Y, Ub = PB[0], PB[1], PB[2]
            pt = PT[hp % 2]
            QZ = (qkT, qz1)
            for e2 in range(2):
                mm(X[:, e2 * 128:(e2 + 1) * 128], qkT[:, 4 + hp, tsl], QZ[e2][:, hp, tsl], True, True,
                   [qkT.c(4 + hp), QZ[e2].c(hp)], [X])
            tt("dve", pt[:], X[:, 0:256].rearrange("p (h t) -> p h t", h=2),
               mask_m[:].unsqueeze(1).to_broadcast([128, 2, 128]), ALU.mult, [X, mask_m], [pt])
            mchk(3)
            yield
            for e2 in range(2):
                h = 2 * hp + e2
                mm(Y[:, e2 * 130:e2 * 130 + 129], pt[:, e2, :], vw[:, h, 0:129], True, False, [pt, vw], [Y])
                mm(Y[:, e2 * 130:e2 * 130 + 129], QZ[e2][:, hp, tsl], Sb[:, hp, 0:129], False, True,
                   [QZ[e2].c(hp), Sb], [Y])
            cp("act", T4[0][:, hp * 260:hp * 260 + 260], Y[:, 0:260], [Y], [T4[0]])
            mchk(4)
            yield
            mm(Ub[:, 0:260], k_tok[:, hp * 128:(hp + 1) * 128],
               vw[:, 2 * hp:2 * hp + 2, :].rearrange("p h c -> p (h c)"), True, True, [k_tok, vw], [Ub])
            tt("dve", S[0:64, hp, 0:129], S[0:64, hp, 0:129], Ub[0:64, 0:129], ALU.add, [S, Ub], [S])
            tt("dve", S[64:128, hp, 0:129], S[64:128, hp, 0:129], Ub[64:128, 130:259], ALU.add, [S, Ub], [S])
            mchk(5)
            yield
        mchk(6)

    def mlstm_tail(li, c):
        tsl = slice(c * 128, (c + 1) * 128)
        osb = T4[0][:, 0:1040].rearrange("p (h c) -> p h c", h=8)
        act(sm_d[:], osb[:, :, 128], AF.Abs, [T4[0]], [sm_d])
        tt("dve", sm_d[:], sm_d[:], gtok[:, c, 8:16], ALU.max, [sm_d, gtok], [sm_d])
        recip(sm_d[:], sm_d[:], [sm_d], [sm_d])
        hv = T4[1][:, 0:1024].rearrange("p (h c) -> p h c", h=8)
        tt("dve", hv, osb[:, :, 0:128], sm_d[:].unsqueeze(2).to_broadcast([128, 8, 128]), ALU.mult,
           [T4[0], sm_d], [T4[1]])
        yield
        for h in range(8):
            P.op("act", lambda e, h=h: e.activation(out=junk[:, 0:128], in_=T4[1][:, h * 128:(h + 1) * 128],
                                                    func=AF.Square, accum_out=sm_ss[:, h:h + 1]),
                 [T4[1]], [junk, sm_ss])
            if h % 4 == 3:
                yield
        ts("dve", sm_ss[:], sm_ss[:], 1.0 / DV, ALU.mult, [sm_ss], [sm_ss], s2=RMS_EPS, op1=ALU.add)
        act(sm_ss[:], sm_ss[:], AF.Ln, [sm_ss], [sm_ss])
        act(sm_ss[:], sm_ss[:], AF.Exp, [sm_ss], [sm_ss], scale=-0.5)
        yield
        tt("dve", hv, hv, sm_ss[:].unsqueeze(2).to_broadcast([128, 8, 128]), ALU.mult, [T4[1], sm_ss], [T4[1]])
        tt("pool", hgb[:, :], T4[1][:, 0:1024], sig_o[:, c, :], ALU.mult, [T4[1], sig_o.c(c)], [hgb])
        yield
        for h in range(8):
            tr(PBT[:, h * 128:(h + 1) * 128], hgb[:, h * 128:(h + 1) * 128], ident_b[:], [hgb, ident_b], [PBT])
        cp("act", h_aT[:, :, tsl], PBT[:, :].rearrange("p (h t) -> p h t", h=8), [PBT], [h_aT])
        yield

    def ssd_gates(li):
        Gq = (Gdt, G[1], G[2], G[3])
        ts("dve", G[1][:], Gdt[:], negA[li][:], ALU.mult, [Gdt, negA[li]], [G[1]])
        P.op("dve", lambda e: e.tensor_tensor_scan(out=G[2][:], data0=cst[0:16, C_RST:C_RST + 512], data1=G[1][:],
                                                   initial=0.0, op0=ALU.mult, op1=ALU.add),
             [cst, G[1]], [G[2]])
        act(G[3][:], G[2][:], AF.Exp, [G[2]], [G[3]])
        yield
        g2v = G[2][:].rearrange("p (c t) -> p c t", c=4)
        g1v = G[1][:].rearrange("p (c t) -> p c t", c=4)
        tt("dve", g1v, g2v[:, :, 127:128].to_broadcast([16, 4, 128]), g2v, ALU.subtract, [G[2]], [G[1]])
        act(G[1][:], G[1][:], AF.Exp, [G[1]], [G[1]])
        yield
        act(sm_cd[:], g2v[:, :, 127], AF.Exp, [G[2]], [sm_cd])
        tt("dve", sm_dg[:], cst[0:16, C_BLK:C_BLK + 16].unsqueeze(1).to_broadcast([16, 4, 16]),
           sm_cd[:].unsqueeze(2).to_broadcast([16, 4, 16]), ALU.mult, [cst, sm_cd], [sm_dg])
        ps_g = PB[6]
        for t4 in range(NTT):
            tsl = slice(t4 * 128, (t4 + 1) * 128)
            for q, gi in enumerate((0, 3, 1, 2)):
                tr(ps_g[:, t4 * 64 + q * 16:t4 * 64 + q * 16 + 16], Gq[gi][:, tsl], cst[0:16, C_ID:C_ID + 16],
                   [Gq[gi], cst], [ps_g])
        mm(ps_g[:, 256:320], cst[0:16, C_ONE:C_ONE + 128], sm_dg[:].rearrange("p c h -> p (c h)"), True, True,
           [cst, sm_dg], [ps_g])
        cp("act", stok[:].rearrange("p t c -> p (t c)"), ps_g[:, 0:256], [ps_g], [stok])
        ts("dve", stok[:, :, 48:64], stok[:, :, 48:64], -1.0, ALU.mult, [stok], [stok])
        cp("act", cd_bc[:].rearrange("p c h -> p (c h)"), ps_g[:, 256:320], [ps_g], [cd_bc])
        yield

    def ssd_chunk(li, c):
        tsl = slice(c * 128, (c + 1) * 128)
        H = Hst[li]
        for kt in range(8):
            tr(PBT[:, kt * 128:(kt + 1) * 128], xbcT[:, kt, tsl], ident_b[:], [xbcT.c(kt), ident_b], [PBT])
        cp("act", xs_tok[:], PBT[:, :], [PBT], [xs_tok])
        yield
        for g in range(4):
            tr(PBT[:, g * 128:(g + 1) * 128], xbcT[:, 8 + g, tsl], ident_b[:], [xbcT.c(8 + g), ident_b], [PBT])
        cp("act", B_tok[:], PBT[:, 0:512], [PBT], [B_tok])
        yield
        xsv = xs_tok[:].rearrange("p (h c) -> p h c", h=16)
        tt("pool", xdt[:], xsv, stok[:, c, 0:16].unsqueeze(2).to_broadcast([128, 16, 64]), ALU.mult,
           [xs_tok, stok], [xdt])
        tt("pool", xdte[:], xdt[:], stok[:, c, 32:48].unsqueeze(2).to_broadcast([128, 16, 64]), ALU.mult,
           [xdt, stok], [xdte])
        yield
        for g in range(4):
            mm(PB[3][:, g * 128:(g + 1) * 128], xbcT[:, 8 + g, tsl], xbcT[:, 12 + g, tsl], True, True,
               [xbcT.c(8 + g), xbcT.c(12 + g)], [PB[3]])
        tt("dve", CBm[:], PB[3][:, :].rearrange("p (g t) -> p g t", g=4),
           mask01.unsqueeze(1).to_broadcast([128, 4, 128]), ALU.mult, [PB[3], cst], [CBm])
        yield
        for g in range(4):
            zg, A_, dec, gg, Yb = Zg[g % 2], PB[4], decb[g % 2], Gg[g % 2], PB[5]
            tt("pool", zg[0:16], G[2][:, tsl].unsqueeze(1).to_broadcast([16, 4, 128]),
               cst[0:16, C_BLK + 4 * g:C_BLK + 4 * g + 4].unsqueeze(2).to_broadcast([16, 4, 128]), ALU.mult,
               [G[2], cst], [zg])
            mm(A_[:, :], ones_f, zg[:].rearrange("p h t -> p (h t)"), True, False,
               [cst, zg], [A_])
            mm(A_[:, :], ident_b[:], negm4[:], False, True, [ident_b, negm4], [A_])
            yield
            for hh in range(4):
                h = 4 * g + hh
                act(dec[:, hh, :], A_[:, hh * 128:(hh + 1) * 128], AF.Exp, [A_, stok], [dec],
                    bias=stok[:, c, 48 + h:49 + h])
            tt("dve", gg[:], dec[:], CBm[:, g, :].unsqueeze(1).to_broadcast([128, 4, 128]), ALU.mult,
               [dec, CBm], [gg])
            yield
            for hh in range(4):
                h = 4 * g + hh
                mm(Yb[:, hh * 64:(hh + 1) * 64], gg[:, hh, :], xdt[:, h, :], True, False, [gg, xdt], [Yb])
                mm(Yb[:, hh * 64:(hh + 1) * 64], xbcT[:, h // 2, tsl], Dm[li][:, h, :], False, True,
                   [xbcT.c(h // 2), Dm[li]], [Yb])
            mm(Yb[:, 256:512], xbcT[:, 12 + g, tsl], Hb[li][:, 4 * g:4 * g + 4, :].rearrange("p h c -> p (h c)"),
               True, True, [xbcT.c(12 + g), Hb[li]], [Yb])
            yv = T4[2][:, g * 256:(g + 1) * 256]
            cp("act", yv, Yb[:, 256:512], [Yb], [T4[2]])
            yield
            yv3 = yv.rearrange("p (h c) -> p h c", h=4)
            tt("dve", yv3, yv3, stok[:, c, 16 + 4 * g:16 + 4 * g + 4].unsqueeze(2).to_broadcast([128, 4, 64]),
               ALU.mult, [T4[2], stok], [T4[2]])
            tt("dve", yv, yv, Yb[:, 0:256], ALU.add, [T4[2], Yb], [T4[2]])
            yield
            U = PB[6]
            mm(U[:, 0:256], B_tok[:, g * 128:(g + 1) * 128],
               xdte[:, 4 * g:4 * g + 4, :].rearrange("p h c -> p (h c)"), True, True, [B_tok, xdte], [U])
            hv = H[:, 4 * g:4 * g + 4, :]
            tt("pool", hv, hv, cd_bc[:, c, 4 * g:4 * g + 4].unsqueeze(2).to_broadcast([128, 4, 64]), ALU.mult,
               [H, cd_bc], [H])
            tt("dve", hv, hv, U[:, 0:256].rearrange("p (h c) -> p h c", h=4), ALU.add, [H, U], [H])
            yield
        cp("act", Hb[li][:], H[:], [H], [Hb[li]])
        yield

    def ssd_tail(li, c):
        tsl = slice(c * 128, (c + 1) * 128)
        tt("pool", T4[3][:, 0:1024], T4[2][:, 0:1024], silu_z[:, c, :], ALU.mult, [T4[2], silu_z.c(c)], [T4[3]])
        yield
        for g in range(4):
            P.op("act", lambda e, g=g: e.activation(out=junk2[:, 0:256], in_=T4[3][:, g * 256:(g + 1) * 256],
                                                    func=AF.Square, accum_out=sm_ss2[:, g:g + 1]),
                 [T4[3]], [junk2, sm_ss2])
        yield
        ts("dve", sm_ss2[:, 0:4], sm_ss2[:, 0:4], 1.0 / 256, ALU.mult, [sm_ss2], [sm_ss2], s2=RMS_EPS, op1=ALU.add)
        act(sm_ss2[:, 0:4], sm_ss2[:, 0:4], AF.Ln, [sm_ss2], [sm_ss2])
        act(sm_ss2[:, 0:4], sm_ss2[:, 0:4], AF.Exp, [sm_ss2], [sm_ss2], scale=-0.5)
        yield
        tt("dve", ynb[:, :].rearrange("p (g c) -> p g c", g=4),
           T4[3][:, 0:1024].rearrange("p (g c) -> p g c", g=4),
           sm_ss2[:, 0:4].unsqueeze(2).to_broadcast([128, 4, 256]), ALU.mult, [T4[3], sm_ss2], [ynb])
        yield
        for kt in range(8):
            tr(PBT[:, kt * 128:(kt + 1) * 128], ynb[:, kt * 128:(kt + 1) * 128], ident_b[:], [ynb, ident_b], [PBT])
        cp("act", h_bT[:, :, tsl], PBT[:, :].rearrange("p (h t) -> p h t", h=8), [PBT], [h_bT])
        yield

    dexp_loaded = [False]

    class _Stop(Exception):
        pass

    def chk(name):
        if stop == name:
            raise _Stop()

    def layer(li):
        try:
            layer_(li)
        except _Stop:
            pass

    def layer_(li):
        o = _off
        pl = prm[li]
        chk("start")
        P.fence([hidT], [xbcT, qkT])
        P.fence(WDb, [v_aug, sig_o, hgb, ynb])
        P.fence([mergedT], [silu_z])
        for s in range(2):
            wb = load_slab(li, "win", s)
            for pr in range(2):
                items = []
                for t4 in (2 * pr, 2 * pr + 1):
                    tq = s * 4 + t4
                    ps = gbank()
                    gemm_A(wb, t4 * 128, 128, xTb, ps)
                    items.append((ps, 128, 4, halo_qk[li], tq, pl[:, o["mcw"] + tq * 4:o["mcw"] + tq * 4 + 4],
                                  pl[:, o["mcb"] + tq:o["mcb"] + tq + 1]))
                accs = conv_pair(items, pl)
                for it, acc in zip(items, accs):
                    tq = it[4]
                    if tq < 4:
                        memset("pool", qkT[64:128, tq, :], 0.0, [qkT.c(tq)])
                        act(qkT[0:64, tq, :], acc[0:64, 0:512], AF.Silu, [acc], [qkT.c(tq)])
                        act(qz1[64:128, tq, :], acc[64:128, 0:512], AF.Silu, [acc], [qz1.c(tq)])
                    else:
                        act(qkT[:, tq, :], acc[:, 0:512], AF.Silu, [acc], [qkT.c(tq)])
        chk("qk")
        for s in range(4):
            wb = load_slab(li, "win", 2 + s)
            for pr in range(2):
                items = []
                for t4 in (2 * pr, 2 * pr + 1):
                    tq = s * 4 + t4
                    ps = gbank()
                    gemm_A(wb, t4 * 128, 128, xTb, ps)
                    items.append((ps, 128, 4, halo_x[li], tq, pl[:, o["scw"] + tq * 4:o["scw"] + tq * 4 + 4],
                                  pl[:, o["scb"] + tq:o["scb"] + tq + 1]))
                accs = conv_pair(items, pl)
                for it, acc in zip(items, accs):
                    act(xbcT[:, it[4], :], acc[:, 0:512], AF.Silu, [acc], [xbcT.c(it[4])])
        chk("xbc")
        for which in range(3):
            for s in range(2):
                wb = load_slab(li, "win", 10 + 2 * which + s)
                for t4 in range(NTT):
                    ps = gbank()
                    for kt in range(KT):
                        mm(ps[:, :], xTb[:, kt, t4 * 128:(t4 + 1) * 128], wb[:, kt, :], kt == 0, kt == KT - 1,
                           [xTb.c(kt), wb], [ps])
                    if which == 0:
                        cp("act", v_aug[:, t4, 4 * s:4 * s + 4, 0:128], ps[:, :].rearrange("p (h c) -> p h c", h=4),
                           [ps], [v_aug.c(t4)])
                    elif which == 1:
                        act(sig_o[:, t4, s * 512:(s + 1) * 512], ps[:, :], AF.Sigmoid, [ps], [sig_o.c(t4)])
                    else:
                        act(silu_z[:, t4, s * 512:(s + 1) * 512], ps[:, :], AF.Silu, [ps], [silu_z.c(t4)])
        for t4 in range(NTT):
            memset("pool", v_aug[:, t4, :, 128:129], 1.0, [v_aug.c(t4)])
        chk("voz")
        wb = WS[ws_ctr[0] % NWS]
        ws_ctr[0] += 1
        wbf = wb[:].rearrange("p k c -> p (k c)")
        P.dma("sp", wbf[:, 0:256], Sd[li]["winm"].h, reads=[Sd[li]["winm"]], writes=[wb])
        wm = wbf[:, 0:256].rearrange("p (k c) -> p k c", k=KT)
        ps_i, ps_f, ps_dt = PB[3], PB[4], PB[5]
        for (ps_, c0, M) in ((ps_i, 0, 8), (ps_f, 8, 8), (ps_dt, 16, 16)):
            for kt in range(KT):
                mm(ps_[0:M, :], wm[:, kt, c0:c0 + M], xTb[:, kt, :], kt == 0, kt == KT - 1, [wb, xTb.c(kt)], [ps_])
        act(Gdt[:], ps_dt[0:16, :], AF.Exp, [ps_dt, pl], [Gdt], bias=pl[0:16, o["dtb"]:o["dtb"] + 1])
        act(Gdt[:], Gdt[:], AF.Ln, [Gdt], [Gdt], bias=1.0)
        chk("mini")
        for _ in mlstm_gates(li, ps_i, ps_f):
            pass
        if stop != "mgates":
            for _ in ssd_gates(li):
                pass

        for st_ in range(NTT + 1):
            gens = []
            if st_ < NTT:
                gens.append(mlstm_chunk(li, st_))
                if stop not in ("mgates", "mlstm"):
                    gens.append(ssd_chunk(li, st_))
            if st_ >= 1:
                gens.append(mlstm_tail(li, st_ - 1))
                if stop not in ("mgates", "mlstm"):
                    gens.append(ssd_tail(li, st_ - 1))
            while gens:
                for g_ in list(gens):
                    try:
                        next(g_)
                    except StopIteration:
                        gens.remove(g_)
        chk("ssd")
        if "h_aT" in dbg:
            dump("h_aT", h_aT, li)
            dump("h_bT", h_bT, li)
        P.fence([silu_z], [mergedT])
        for br, (pname, gslab, hT) in enumerate((("pa", 6, h_aT), ("pb", 8, h_bT))):
            for half in range(2):
                wp = load_slab(li, pname, half)
                wg = load_slab(li, "win", gslab + half)
                ps1s = []
                for t4 in range(4):
                    ps1 = PB[t4]
                    gemm_A(wp, t4 * 128, 128, hT, ps1)
                    ps1s.append(ps1)
                for t4 in range(4):
                    kt_o = half * 4 + t4
                    ps1 = ps1s[t4]
                    ps2 = PB[4 + t4 % 2]
                    gemm_A(wg, t4 * 128, 128, xTb, ps2)
                    sgb = T4[kt_o % 4]
                    sg = sgb[:, 0:512]
                    act(sg, ps2[:, :], AF.Sigmoid, [ps2], [sgb])
                    if br == 0:
                        tt("dve", mergedT[:, kt_o, :], ps1[:, :], sg, ALU.mult, [ps1, sgb], [mergedT.c(kt_o)])
                    else:
                        tt("dve", sg, ps1[:, :], sg, ALU.mult, [ps1, sgb], [sgb])
                        tt("pool", mergedT[:, kt_o, :], mergedT[:, kt_o, :], sg, ALU.add,
                           [mergedT.c(kt_o), sgb], [mergedT.c(kt_o)])
        chk("merge")
        for half in range(2):
            wb = load_slab(li, "wo", half)
            for t4 in range(4):
                kt_o = half * 4 + t4
                ps = gbank()
                gemm_A(wb, t4 * 128, 128, mergedT, ps)
                stt(xT[:, kt_o, :], xT[:, kt_o, :], ALPHA, ps[:, :], ALU.mult, ALU.add, [xT.c(kt_o), ps],
                    [xT.c(kt_o)])
        layernorm(pl[:, o["ln1g"]:o["ln1g"] + 8], pl[:, o["ln1b"]:o["ln1b"] + 8], pl)
        if "x1" in dbg:
            dump("x1", xT, li)
        chk("ln1")
        P.fence([xbcT, qkT], [hidT])
        P.fence([v_aug, sig_o, hgb, ynb], WDb)
        for s in range(11):
            wb = load_slab(li, "wup", s)
            for jj in range(2):
                j = 2 * s + jj
                M = 128 if j < 21 else 64
                base = jj * 256
                items = []
                for gv in range(2):
                    ps = gbank()
                    gemm_A(wb, base + gv * M, M, xTb, ps)
                    hc = 2 * j + gv
                    wof = o["fcw"] + hc * 3
                    items.append((ps, M, 3, halo_f[li], hc, pl[:, wof:wof + 3], pl[:, o["fcb"] + hc:o["fcb"] + hc + 1]))
                accs = conv_pair(items, pl)
                act(accs[0][0:M, 0:512], accs[0][0:M, 0:512], AF.Silu, [accs[0]], [accs[0]])
                tt("pool", hidT[0:M, j, :], accs[0][0:M, 0:512], accs[1][0:M, 0:512], ALU.mult, [accs[0], accs[1]],
                   [hidT.c(j)])
        chk("ffnup")
        memset("pool", hidT[64:128, NJ - 1, :], 0.0, [hidT.c(NJ - 1)])
        for q in range(8):
            wd = WDb[q % 4]
            P.dma("sp", wd[:].rearrange("p j c -> p (j c)"), Sd[li]["wdn"].h[q], reads=[Sd[li]["wdn"].c(q)],
                  writes=[wd])
            kt_o = q
            ps = gbank()
            for j in range(NJ):
                mm(ps[:, :], wd[:, j, :], hidT[:, j, :], j == 0, j == NJ - 1, [wd, hidT.c(j)], [ps])
            stt(xT[:, kt_o, :], xT[:, kt_o, :], ALPHA, ps[:, :], ALU.mult, ALU.add, [xT.c(kt_o), ps],
                [xT.c(kt_o)])
        layernorm(pl[:, o["ln2g"]:o["ln2g"] + 8], pl[:, o["ln2b"]:o["ln2b"] + 8], pl)

    def dump(name, buf, li):
        key = f"dbg_{name}{li}"
        if key in dbg_out:
            return
        d = nc.dram_tensor(key, [128, KT * TB], buf.h.dtype if hasattr(buf.h, "dtype") else F32,
                           kind="ExternalOutput").ap()
        dbg_out[key] = d
        P.dma("sp", d, buf[:].rearrange("p k t -> p (k t)"), reads=[buf], sem_buf=buf)

    for u in range(n_units):
        if u in seq_starts:
            for li in range(nlayers):
                memset("pool", halo_qk[li][:], 0.0, [halo_qk[li]])
                memset("pool", halo_x[li][:], 0.0, [halo_x[li]])
                memset("pool", halo_f[li][:], 0.0, [halo_f[li]])
                memset("pool", Bc[li][:], 0.0, [Bc[li]])
                memset("pool", Rc[li][:], NEG_BIG, [Rc[li]])
                memset("pool", Sst[li][:], 0.0, [Sst[li]])
                memset("pool", Hst[li][:], 0.0, [Hst[li]])
                memset("pool", Hb[li][:], 0.0, [Hb[li]])
        P.fence([xbcT, qkT, hidT], [xin])
        P.dma("sp", xin[:].rearrange("p k t -> p (k t)"), xT_d[u], writes=[xin])
        if entry_ln:
            layernorm(prm[0][:, _off["inlg"]:_off["inlg"] + 8], prm[0][:, _off["inlb"]:_off["inlb"] + 8], prm[0],
                      src=xin)
        else:
            for kt in range(KT):
                cp("pool", xT[:, kt, :], xin[:, kt, :], [xin.c(kt)], [xT.c(kt)])
                cp("act", xTb[:, kt, :], xin[:, kt, :], [xin.c(kt)], [xTb.c(kt)])
        P.fence([xin], [xbcT, qkT, hidT])
        for li in range(nlayers):
            layer(li)
        P.dma("sp", oT_d[u], xT[:].rearrange("p k t -> p (k t)"), reads=[xT], sem_buf=xT)

    P.emit()
    es.close()
    return nc, list(dbg_out.keys())


def _x_units(x, core):
    units = []
    for s in range(2):
        b = 2 * core + s
        for blk in range(NBLK):
            xb = x[b, blk * TB:(blk + 1) * TB, :]
            a = xb.T.reshape(KT, 128, TB).transpose(1, 0, 2)
            units.append(np.ascontiguousarray(a).reshape(128, KT * TB))
    return np.stack(units)


def _units_to_out(o_units, out, core):
    for s in range(2):
        b = 2 * core + s
        for blk in range(NBLK):
            a = o_units[s * NBLK + blk].reshape(128, KT, TB).transpose(1, 0, 2).reshape(D, TB)
            out[b, blk * TB:(blk + 1) * TB, :] = a.T


_CACHE = {}


def _get_program(key, *args):
    if key not in _CACHE:
        _CACHE[key] = build_program(*args)
    return _CACHE[key]


FUSED = True


def kernel(**inp):
    inp = {k: np.asarray(v, np.float32) for k, v in inp.items()}
    x = inp["x"]
    ncores = 8
    cst = _make_cst()
    lay = [_layer_arrays(inp, l) for l in range(DEPTH)]
    n_units = 2 * NBLK
    seq_starts = {0, NBLK}
    out = np.zeros((BATCH, SEQ, D), np.float32)
    if FUSED:
        nc, _ = _get_program("fused", n_units, seq_starts, DEPTH, True)
        in_maps = []
        for c in range(ncores):
            m = {"xT": _x_units(x, c), "cst": cst}
            for l in range(DEPTH):
                for n, _sh in WSHAPES:
                    m[f"{n}{l}"] = lay[l][n]
                m[f"prm{l}"] = lay[l]["prm"]
                m[f"dexp{l}"] = lay[l]["dexp"]
            in_maps.append(m)
        res = run_bass_kernel_spmd(nc, in_maps, core_ids=list(range(ncores)))
        for c in range(ncores):
            _units_to_out(res.results[c]["oT"], out, c)
        return out
    cur = [_x_units(x, c) for c in range(ncores)]
    for l in range(DEPTH):
        nc, _ = _get_program(("layer", l == 0), n_units, seq_starts, 1, l == 0)
        in_maps = []
        for c in range(ncores):
            m = {"xT": cur[c], "cst": cst}
            for n, _sh in WSHAPES:
                m[f"{n}0"] = lay[l][n]
            m["prm0"] = lay[l]["prm"]
            m["dexp0"] = lay[l]["dexp"]
            in_maps.append(m)
        res = run_bass_kernel_spmd(nc, in_maps, core_ids=list(range(ncores)))
        cur = [np.asarray(res.results[c]["oT"], np.float32) for c in range(ncores)]
    for c in range(ncores):
        _units_to_out(cur[c], out, c)
    return out
```
